# Optimizing a Trainium2 kernel written in Bass

```python
import math
import jax
import jax.numpy as jnp
from jax import lax
import numpy as np


D_MODEL = 1024
BATCH = 8
SEQ = 2048
DEPTH = 2

GRID_W = 64
CTX_LEN = 256
N_MIXERS = 2
EPS = 1e-6
DIFF_HEADS = 8
DIFF_HEAD_DIM = D_MODEL // (2 * DIFF_HEADS)
ROPE_THETA = 10000.0
ROPE_AXIS_DIM = DIFF_HEAD_DIM // 2
ROPE_FREQS = ROPE_AXIS_DIM // 2
Q_BLOCK = 128
D_RNN = D_MODEL
LRU_BLOCKS = 8
LRU_BW = D_RNN // LRU_BLOCKS
CONV_W = 4
LRU_C = 8.0
N_EXPERTS = 16
EC_CAPACITY = 2
D_EXPERT = 2816
N_ATTN = (DEPTH + 1) // 2
N_LRU = DEPTH // 2

kernel_name = 'hybrid_diffattn_rglru_ecmoe_dit'

F32 = jnp.float32


def rmsnorm(x, g):
    xf = x.astype(F32)
    y = xf * lax.rsqrt(jnp.mean(xf * xf, axis=-1, keepdims=True) + EPS)
    return (y * g.astype(F32)).astype(x.dtype)


def modulate(x, shift, scale):
    return x * (1.0 + scale) + shift


def axial_rope_tables(n):
    rows = n // GRID_W
    row = jnp.repeat(jnp.arange(rows), GRID_W).astype(F32)
    col = jnp.tile(jnp.arange(GRID_W), rows).astype(F32)
    inv = ROPE_THETA ** (-(jnp.arange(ROPE_FREQS, dtype=F32) * 2.0) / ROPE_AXIS_DIM)
    ang = jnp.concatenate([row[:, None] * inv, col[:, None] * inv], axis=-1)
    return jnp.cos(ang), jnp.sin(ang)


def apply_axial_rope(x, cos, sin):
    n = x.shape[1]
    xr = x.astype(F32).reshape(*x.shape[:-1], 2, 2, ROPE_FREQS)
    x1, x2 = xr[..., 0, :], xr[..., 1, :]
    c = cos.reshape(n, 1, 2, ROPE_FREQS)
    s = sin.reshape(n, 1, 2, ROPE_FREQS)
    out = jnp.stack([x1 * c - x2 * s, x2 * c + x1 * s], axis=-2)
    return out.reshape(x.shape).astype(x.dtype)


def diff_attn_core(q, k, v, lam):
    s = jnp.einsum('bqhd,bkhd->bhqk', q.astype(F32), k.astype(F32))
    p = jax.nn.softmax(s, axis=-1)
    b_, _, lq, lk = p.shape
    p = p.reshape(b_, DIFF_HEADS, 2, lq, lk)
    w = p[:, :, 0] - lam * p[:, :, 1]
    return jnp.einsum('bhqk,bkhe->bqhe', w, v.astype(F32))


def diff_attention(n_lat, n_ctx, w_qkv, lq1, lk1, lq2, lk2, subln_g, w_o, lambda_init, ctx_out):
    b_, n, _ = n_lat.shape
    H, dh = DIFF_HEADS, DIFF_HEAD_DIM
    lam = (jnp.exp(jnp.sum(lq1.astype(F32) * lk1.astype(F32)))
           - jnp.exp(jnp.sum(lq2.astype(F32) * lk2.astype(F32))) + lambda_init)

    def project(h):
        L = h.shape[1]
        q, k, v = jnp.split(h @ w_qkv, 3, axis=-1)
        return (q.reshape(b_, L, 2 * H, dh) * (dh ** -0.5),
                k.reshape(b_, L, 2 * H, dh),
                v.reshape(b_, L, H, 2 * dh))

    q_l, k_l, v_l = project(n_lat)
    q_c, k_c, v_c = project(n_ctx)
    cos, sin = axial_rope_tables(n)
    q_l = apply_axial_rope(q_l, cos, sin)
    k_l = apply_axial_rope(k_l, cos, sin)
    k_all = jnp.concatenate([k_l, k_c], axis=1)
    v_all = jnp.concatenate([v_l, v_c], axis=1)
    nb = n // Q_BLOCK
    q_blocks = q_l.reshape(b_, nb, Q_BLOCK, 2 * H, dh).swapaxes(0, 1)
    o_blocks = lax.map(lambda qb: diff_attn_core(qb, k_all, v_all, lam), q_blocks)
    o_l = o_blocks.swapaxes(0, 1).reshape(b_, n, H, 2 * dh)

    def finish(o):
        o = rmsnorm(o, subln_g) * (1.0 - lambda_init)
        return o.reshape(b_, o.shape[1], D_MODEL).astype(n_lat.dtype) @ w_o

    y_l = finish(o_l)
    y_c = finish(diff_attn_core(q_c, k_c, v_c, lam)) if ctx_out else None
    return y_l, y_c


def centred_depthwise_conv(x, w, b):
    left = CONV_W // 2
    right = CONV_W - 1 - left
    y = lax.conv_general_dilated(x, w[:, None, :].astype(x.dtype), window_strides=(1,),
                                 padding=[(left, right)],
                                 dimension_numbers=('NWC', 'WIO', 'NWC'),
                                 feature_group_count=x.shape[-1])
    return y + b.astype(x.dtype)


def linear_scan(a, b, h0):
    def combine(l, r):
        return l[0] * r[0], r[0] * l[1] + r[1]
    A, Bc = lax.associative_scan(combine, (a, b), axis=1)
    return A * h0[:, None, :] + Bc


def rglru_block(n_lat, n_ctx, w_in, b_in, conv_w, conv_b, w_gates, b_gates, lam_p, w_out, ctx_out):
    def gate_terms(xc, d):
        b_, L, _ = xc.shape
        xf = xc.astype(F32)
        g = jnp.einsum('blkc,kcd->blkd', xf.reshape(b_, L, LRU_BLOCKS, LRU_BW),
                       w_gates[d].astype(F32)) + b_gates[d].astype(F32)
        r = jax.nn.sigmoid(g[..., :LRU_BW]).reshape(b_, L, D_RNN)
        i = jax.nn.sigmoid(g[..., LRU_BW:]).reshape(b_, L, D_RNN)
        log_a = -LRU_C * r * jax.nn.softplus(-lam_p[d].astype(F32))
        a = jnp.exp(log_a)
        bt = jnp.sqrt(-jnp.expm1(2.0 * log_a)) * (i * xf)
        return a, bt

    y_l, xr_l = jnp.split(n_lat @ w_in + b_in, 2, axis=-1)
    x_l = centred_depthwise_conv(xr_l, conv_w, conv_b)
    if ctx_out:
        y_c, xr_c = jnp.split(n_ctx @ w_in + b_in, 2, axis=-1)
    else:
        xr_c = n_ctx @ w_in[:, D_RNN:] + b_in[D_RNN:]
    x_c = centred_depthwise_conv(xr_c, conv_w, conv_b)

    h_l = 0.0
    h_c = 0.0
    for d in range(2):
        a_c, b_c = gate_terms(x_c, d)
        a_l, b_l = gate_terms(x_l, d)
        if d == 1:
            a_c, b_c, a_l, b_l = (jnp.flip(t, axis=1) for t in (a_c, b_c, a_l, b_l))
        s_c = linear_scan(a_c, b_c, jnp.zeros_like(b_c[:, 0]))
        s_l = linear_scan(a_l, b_l, s_c[:, -1])
        if d == 1:
            s_l = jnp.flip(s_l, axis=1)
            s_c = jnp.flip(s_c, axis=1)
        h_l = h_l + s_l
        if ctx_out:
            h_c = h_c + s_c
    out_l = (jax.nn.gelu(y_l.astype(F32)) * h_l).astype(n_lat.dtype) @ w_out
    out_c = ((jax.nn.gelu(y_c.astype(F32)) * h_c).astype(n_ctx.dtype) @ w_out) if ctx_out else None
    return out_l, out_c


def expert_choice_moe(h, w_router, w_gate_up, w_down):
    b_, L, _ = h.shape
    cap = EC_CAPACITY * L // N_EXPERTS
    logits = jnp.einsum('bld,de->bel', h.astype(F32), w_router.astype(F32))
    probs = jax.nn.softmax(logits, axis=1)
    gate, idx = lax.top_k(probs, cap)
    bidx = jnp.arange(b_)[:, None, None]
    xg = h[bidx, idx]
    gu = jnp.einsum('becd,edf->becf', xg, w_gate_up)
    g_, u_ = jnp.split(gu, 2, axis=-1)
    y = jnp.einsum('becf,efd->becd', jax.nn.silu(g_) * u_, w_down)
    y = (y.astype(F32) * gate[..., None]).astype(h.dtype)
    return jnp.zeros_like(h).at[bidx, idx].add(y)


def setup_inputs(seed: int = 0) -> dict:
    key = jax.random.key(seed)
    ks = jax.random.split(key, 32)
    D = D_MODEL

    def nrm(k, shape, fan_in):
        return jax.random.normal(k, shape, F32) * (fan_in ** -0.5)

    def small(k, shape, s=0.02):
        return jax.random.normal(k, shape, F32) * s

    u = jax.random.uniform(ks[20], (N_LRU, 2, D_RNN), F32, minval=0.9, maxval=0.999)
    base = u ** (1.0 / LRU_C)
    lru_lambda = jnp.log(base) - jnp.log1p(-base)

    return {
        'x': jax.random.normal(ks[0], (BATCH, SEQ, D), F32),
        'c': jax.random.normal(ks[1], (BATCH, D), F32),
        'ctx': jax.random.normal(ks[2], (BATCH, CTX_LEN, D), F32),
        'c_ctx': jax.random.normal(ks[3], (D,), F32),
        'ada_w': nrm(ks[4], (DEPTH, D, 6 * D), D),
        'ada_b': small(ks[5], (DEPTH, 6 * D)),
        'norm1_g': 1.0 + small(ks[6], (DEPTH, D)),
        'norm2_g': 1.0 + small(ks[7], (DEPTH, D)),
        'final_g': 1.0 + small(ks[8], (D,)),
        'attn_w_qkv': nrm(ks[9], (N_ATTN, D, 3 * D), D),
        'attn_lq1': small(ks[10], (N_ATTN, DIFF_HEAD_DIM), 0.1),
        'attn_lk1': small(ks[11], (N_ATTN, DIFF_HEAD_DIM), 0.1),
        'attn_lq2': small(ks[12], (N_ATTN, DIFF_HEAD_DIM), 0.1),
        'attn_lk2': small(ks[13], (N_ATTN, DIFF_HEAD_DIM), 0.1),
        'attn_subln_g': 1.0 + small(ks[14], (N_ATTN, 2 * DIFF_HEAD_DIM)),
        'attn_w_o': nrm(ks[15], (N_ATTN, D, D), D),
        'lru_w_in': nrm(ks[16], (N_LRU, D, 2 * D_RNN), D),
        'lru_b_in': small(ks[17], (N_LRU, 2 * D_RNN)),
        'lru_conv_w': nrm(ks[18], (N_LRU, CONV_W, D_RNN), CONV_W),
        'lru_conv_b': small(ks[19], (N_LRU, D_RNN)),
        'lru_w_gates': nrm(ks[21], (N_LRU, 2, LRU_BLOCKS, LRU_BW, 2 * LRU_BW), LRU_BW),
        'lru_b_gates': small(ks[22], (N_LRU, 2, LRU_BLOCKS, 2 * LRU_BW)),
        'lru_lambda': lru_lambda,
        'lru_w_out': nrm(ks[23], (N_LRU, D_RNN, D), D_RNN),
        'moe_w_router': nrm(ks[24], (DEPTH, D, N_EXPERTS), D),
        'moe_w_gate_up': nrm(ks[25], (DEPTH, N_EXPERTS, D, 2 * D_EXPERT), D),
        'moe_w_down': nrm(ks[26], (DEPTH, N_EXPERTS, D_EXPERT, D), D_EXPERT),
    }


def reference(x, c, ctx, c_ctx, ada_w, ada_b, norm1_g, norm2_g, final_g,
              attn_w_qkv, attn_lq1, attn_lk1, attn_lq2, attn_lk2, attn_subln_g, attn_w_o,
              lru_w_in, lru_b_in, lru_conv_w, lru_conv_b, lru_w_gates, lru_b_gates, lru_lambda, lru_w_out,
              moe_w_router, moe_w_gate_up, moe_w_down):
    h_lat = x
    h_ctx = ctx
    for i in range(DEPTH):
        last = i == DEPTH - 1
        mod_l = (jax.nn.silu(c) @ ada_w[i] + ada_b[i])[:, None, :]
        mod_c = jax.nn.silu(c_ctx) @ ada_w[i] + ada_b[i]
        sh1, sc1, g1, sh2, sc2, g2 = jnp.split(mod_l, 6, axis=-1)
        csh1, csc1, cg1, csh2, csc2, cg2 = jnp.split(mod_c, 6, axis=-1)

        n_l = modulate(rmsnorm(h_lat, norm1_g[i]), sh1, sc1)
        n_c = modulate(rmsnorm(h_ctx, norm1_g[i]), csh1, csc1)
        j = i // N_MIXERS
        if i % N_MIXERS == 0:
            lambda_init = 0.8 - 0.6 * math.exp(-0.3 * i)
            y_l, y_c = diff_attention(n_l, n_c, attn_w_qkv[j], attn_lq1[j], attn_lk1[j],
                                      attn_lq2[j], attn_lk2[j], attn_subln_g[j], attn_w_o[j],
                                      lambda_init, not last)
        else:
            y_l, y_c = rglru_block(n_l, n_c, lru_w_in[j], lru_b_in[j], lru_conv_w[j], lru_conv_b[j],
                                   lru_w_gates[j], lru_b_gates[j], lru_lambda[j], lru_w_out[j], not last)
        h_lat = h_lat + (g1 * y_l).astype(h_lat.dtype)
        if not last:
            h_ctx = h_ctx + (cg1 * y_c).astype(h_ctx.dtype)

        m_l = modulate(rmsnorm(h_lat, norm2_g[i]), sh2, sc2)
        h_lat = h_lat + (g2 * expert_choice_moe(m_l, moe_w_router[i], moe_w_gate_up[i],
                                                moe_w_down[i])).astype(h_lat.dtype)
        if not last:
            m_c = modulate(rmsnorm(h_ctx, norm2_g[i]), csh2, csc2)
            h_ctx = h_ctx + (cg2 * expert_choice_moe(m_c, moe_w_router[i], moe_w_gate_up[i],
                                                    moe_w_down[i])).astype(h_ctx.dtype)
    return rmsnorm(h_lat, final_g)
```

```python
import math
import numpy as np
import ml_dtypes
import concourse.bass as bass
import concourse.mybir as mybir
from concourse.bass_utils import run_bass_kernel_spmd

F32 = mybir.dt.float32
BF16 = mybir.dt.bfloat16
I32 = mybir.dt.int32
U32 = mybir.dt.uint32
AF = mybir.ActivationFunctionType
ALU = mybir.AluOpType
AX = mybir.AxisListType

ENGS = ["sync", "scalar", "vector", "gpsimd", "tensor"]
D = 1024
NTL, NTC, NT = 16, 2, 18
EPS = 1e-6
NE = 16
FE = 2816
DT_SIZE = {F32: 4, BF16: 2, I32: 4, U32: 4}


class Prog:
    def __init__(self, nc, same_engine_sync=("vector", "scalar", "gpsimd")):
        self.nc = nc
        self.stream = {e: [] for e in ENGS}
        self.ecount = {e: 0 for e in ENGS}
        self.chan = {}
        self.last_w = {}
        self.readers = {}
        self.waited = {e: {} for e in ENGS}
        self.same_engine_sync = set(same_engine_sync)
        self.sems = {}

    def _deps(self, reads, writes):
        toks = []
        for r in reads:
            t = self.last_w.get(r)
            if t is not None:
                toks.append(t)
        for w in writes:
            t = self.last_w.get(w)
            if t is not None:
                toks.append(t)
            toks.extend(self.readers.get(w, []))
        return toks

    def _record(self, tok, reads, writes):
        for r in reads:
            self.readers.setdefault(r, []).append(tok)
        for w in writes:
            self.last_w[w] = tok
            self.readers[w] = []

    def _emit_waits(self, eng, toks, all_same=False):
        need = {}
        for (k, v) in toks:
            if k == ("e", eng) and not all_same and eng not in self.same_engine_sync:
                continue
            if v > need.get(k, 0):
                need[k] = v
        for k, v in need.items():
            if self.waited[eng].get(k, 0) >= v:
                continue
            self.waited[eng][k] = v
            self.stream[eng].append(("wait", k, v))

    def op(self, eng, fn, reads=(), writes=()):
        reads = list(reads); writes = list(writes)
        self._emit_waits(eng, self._deps(reads, writes))
        self.ecount[eng] += 1
        tok = (("e", eng), self.ecount[eng])
        self.stream[eng].append(("ins", fn, ("e", eng), 1))
        self._record(tok, reads, writes)
        return tok

    def dma(self, eng, fn, chan, reads=(), writes=()):
        reads = list(reads); writes = list(writes)
        self._emit_waits(eng, self._deps(reads, writes))
        c = self.chan.setdefault(chan, [("c", chan), 0])
        c[1] += 16
        tok = (c[0], c[1])
        self.stream[eng].append(("ins", fn, c[0], 16))
        self._record(tok, reads, writes)
        return tok

    def _all_toks(self):
        toks = [(("e", e), self.ecount[e]) for e in ENGS if self.ecount[e] > 0]
        toks += [(c[0], c[1]) for c in self.chan.values()]
        return toks

    def barrier(self):
        toks = self._all_toks()
        for e in ENGS:
            self._emit_waits(e, toks, all_same=True)
        self.last_w = {}
        self.readers = {}

    def wait_all(self, eng):
        self._emit_waits(eng, self._all_toks(), all_same=True)

    def emit(self):
        nc = self.nc
        keys = [("e", e) for e in ENGS if self.ecount[e] > 0] + [c[0] for c in self.chan.values()]
        for i, k in enumerate(keys):
            self.sems[k] = nc.alloc_semaphore(name="sm%d" % i)
        sems = self.sems
        stream = self.stream

        def run(eng_name):
            def f(eng):
                for item in stream[eng_name]:
                    if item[0] == "wait":
                        eng.wait_ge(sems[item[1]], item[2])
                    else:
                        _, fn, k, inc = item
                        fn(eng).then_inc(sems[k], inc)
            return f

        with nc.Block() as block:
            block.sync(run("sync"))
            block.scalar(run("scalar"))
            block.vector(run("vector"))
            block.gpsimd(run("gpsimd"))
            block.tensor(run("tensor"))


class Arena:
    def __init__(self, nc, base=16512, top=229344):
        self.nc = nc; self.base = base; self.top = top; self.off = base; self.cnt = 0

    def mark(self):
        return self.off

    def reset(self, to=None):
        self.off = self.base if to is None else to

    def tile(self, shape, dt, name="t"):
        n = 1
        for s in shape[1:]:
            n *= s
        nbytes = (n * DT_SIZE[dt] + 31) // 32 * 32
        assert self.off + nbytes <= self.top, ("SBUF overflow", name, self.off, nbytes)
        self.cnt += 1
        t = self.nc.alloc_sbuf_tensor_at("%s_%d" % (name, self.cnt), list(shape), dt, offset=self.off)
        self.off += nbytes
        return t


def rope_tables():
    n = 2048
    row = np.repeat(np.arange(n // 64), 64).astype(np.float32)
    col = np.tile(np.arange(64), n // 64).astype(np.float32)
    inv = (np.float32(10000.0) ** (-(np.arange(16, dtype=np.float32) * np.float32(2.0)) / np.float32(32))).astype(np.float32)
    ang = np.concatenate([row[:, None] * inv, col[:, None] * inv], axis=-1).astype(np.float32)
    cos = np.cos(ang).astype(np.float32); sin = np.sin(ang).astype(np.float32)
    C = np.zeros((128, n), np.float32); S = np.zeros((128, n), np.float32)
    perm = np.zeros((128, 128), np.float32)
    for p in range(128):
        d = p % 64
        a = d // 32; half = (d % 32) // 16; f = d % 16
        C[p] = cos[:, a * 16 + f]
        S[p] = sin[:, a * 16 + f] * (-1.0 if half == 0 else 1.0)
        partner = p + 16 if half == 0 else p - 16
        perm[partner, p] = 1.0
    return C, S, perm


def build_program(debug=False, stop_after=None, attn_stop=None):
    nc = bass.Bass("TRN2", target_bir_lowering=False)
    P = Prog(nc)
    A = Arena(nc)

    def din(name, shape, dt=F32):
        return nc.dram_tensor(name, list(shape), dt, kind="ExternalInput").ap()

    x_in = din("x", [2048, D]); ctx_in = din("ctx", [256, D]); c_in = din("c", [D]); cctx_in = din("c_ctx", [D])
    ada_w = din("ada_w", [2, D, 6 * D]); ada_b = din("ada_b", [2, 6 * D])
    norm1_g = din("norm1_g", [2, D]); norm2_g = din("norm2_g", [2, D]); final_g = din("final_g", [1, D])
    w_qkv = din("attn_w_qkv", [D, 3 * D])
    lq1 = din("attn_lq1", [1, 64]); lk1 = din("attn_lk1", [1, 64]); lq2 = din("attn_lq2", [1, 64]); lk2 = din("attn_lk2", [1, 64])
    subln_g = din("attn_subln_g", [1, 128]); w_o = din("attn_w_o", [D, D])
    lru_w_in = din("lru_w_in", [D, 2 * D]); lru_b_in = din("lru_b_in", [2 * D])
    conv_w = din("lru_conv_w", [4, D]); conv_b = din("lru_conv_b", [D])
    w_gates = din("lru_w_gates", [2, 8, 128, 256]); b_gates = din("lru_b_gates", [2, 8, 256])
    lru_lam = din("lru_lambda", [2, D]); lru_w_out = din("lru_w_out", [D, D])
    w_router = din("moe_w_router", [2, D, NE]); w_gu = din("moe_w_gate_up", [2, NE, D, 2 * FE]); w_dn = din("moe_w_down", [2, NE, FE, D])
    rope_c = din("rope_c", [128, 2048]); rope_s = din("rope_s", [128, 2048]); perm_in = din("perm", [128, 128])
    ident_in = din("ident", [128, 128])
    out = nc.dram_tensor("out", [2048, D], F32, kind="ExternalOutput").ap()
    h = nc.dram_tensor("h_scr", [2304, D], F32, kind="ExternalOutput" if debug else "Internal").ap()
    modrow = nc.dram_tensor("modrow", [2, 2, 6 * D], F32, kind="ExternalOutput" if debug else "Internal").ap()
    mbf = nc.dram_tensor("mbf", [2304, D], BF16).ap()

    psb = [nc.alloc_psum_tensor("psb%d" % i, [128, 512], F32) for i in range(8)]

    def psbf(i):
        return psb[i][:].bitcast(BF16).rearrange("p (j n) -> p j n", j=8)

    def mm(o, lhsT, rhs, start, stop, r, w):
        P.op("tensor", lambda e: e.matmul(o, lhsT=lhsT, rhs=rhs, start=start, stop=stop), r, w)

    def tr(o, i, idn, r, w):
        P.op("tensor", lambda e: e.transpose(o, i, idn), r, w)

    def act(o, i, func, r, w, **kw):
        P.op("scalar", lambda e: e.activation(out=o, in_=i, func=func, **kw), r, w)

    def tt(eng, o, a, b, op, r, w):
        P.op(eng, lambda e: e.tensor_tensor(out=o, in0=a, in1=b, op=op), r, w)

    def ts(eng, o, a, s1, s2, op0, op1, r, w):
        if s2 is None:
            P.op(eng, lambda e: e.tensor_scalar(out=o, in0=a, scalar1=s1, scalar2=None, op0=op0), r, w)
        else:
            P.op(eng, lambda e: e.tensor_scalar(out=o, in0=a, scalar1=s1, scalar2=s2, op0=op0, op1=op1), r, w)

    def stt(eng, o, a, s, b, op0, op1, r, w):
        P.op(eng, lambda e: e.scalar_tensor_tensor(out=o, in0=a, scalar=s, in1=b, op0=op0, op1=op1), r, w)

    def cp(eng, o, i, r, w):
        if eng == "scalar":
            P.op(eng, lambda e: e.copy(out=o, in_=i), r, w)
        else:
            P.op(eng, lambda e: e.tensor_copy(out=o, in_=i), r, w)

    def dma(eng, o, i, chan, r, w, **kw):
        P.dma(eng, lambda e: e.dma_start(out=o, in_=i, **kw), chan, r, w)

    def cast_rows(dst3, src2, col0, ncols, chan, dcol0=0):
        J = src2.shape[0] // 128
        for j in range(J):
            for c in range(0, ncols, 1024):
                w_ = min(1024, ncols - c)
                dma("gpsimd", dst3[:, j, dcol0 + c:dcol0 + c + w_], src2[j * 128:(j + 1) * 128, col0 + c:col0 + c + w_], chan, [], [chan])

    def bcload(tile_, row_ap, n, chan, eng="sync"):
        dma(eng, tile_[:], row_ap.to_broadcast([128, n]), chan, [], [chan])

    ident_bf = A.tile([128, 128], BF16, "identb")
    ident_f = A.tile([128, 128], F32, "identf")
    dma("gpsimd", ident_bf[:], ident_in, "identb", [], ["identb"])
    dma("sync", ident_f[:], ident_in, "identf", [], ["identf"])
    base_mark = A.mark()

    def rstd_from_ss(ss, rstd, n, key):
        ts("vector", rstd, ss, 1.0 / n, EPS, ALU.mult, ALU.add, [key + "ss"], [key + "rstd"])
        act(rstd, rstd, AF.Sqrt, [key + "rstd"], [key + "rstd"])
        P.op("vector", lambda e: e.reciprocal(out=rstd, in_=rstd), [key + "rstd"], [key + "rstd"])

    def phase_mod():
        A.reset(base_mark)
        cl = A.tile([128, 8], F32, "cl"); cc = A.tile([128, 8], F32, "cc")
        sct = A.tile([128, 8, 2], F32, "sct")
        adab = A.tile([2, 6 * D], F32, "adab"); modsb = A.tile([2, 6 * D], F32, "modsb")
        wa = [A.tile([128, 8, 512], F32, "wa") for _ in range(3)]
        dma("sync", cl[:], c_in.rearrange("(j p) -> p j", p=128), "cl", [], ["cl"], allow_slow_non_contiguous=True)
        dma("sync", cc[:], cctx_in.rearrange("(j p) -> p j", p=128), "cc", [], ["cc"], allow_slow_non_contiguous=True)
        act(sct[:, :, 0], cl[:], AF.Silu, ["cl"], ["sct0"])
        act(sct[:, :, 1], cc[:], AF.Silu, ["cc"], ["sct1"])
        it = 0
        for l in range(2):
            dma("sync", adab[:], ada_b[l:l + 1, :].to_broadcast([2, 6 * D]), "adab", [], ["adab"])
            awv = ada_w[l].rearrange("(j p) n -> p j n", p=128)
            for n in range(12):
                s = it % 3; b = it % 2; it += 1
                dma("sync" if it % 2 else "scalar", wa[s][:], awv[:, :, n * 512:(n + 1) * 512], "wa%d" % s, [], ["wa%d" % s])
                for j in range(8):
                    mm(psb[b][0:2, :], sct[:, j, :], wa[s][:, j, :], j == 0, j == 7, ["sct0", "sct1", "wa%d" % s], ["ps%d" % b])
                tt("vector", modsb[:, n * 512:(n + 1) * 512], psb[b][0:2, :], adab[:, n * 512:(n + 1) * 512], ALU.add,
                   ["ps%d" % b, "adab"], ["modsb"])
            dma("sync", modrow[l], modsb[:], "modsb", ["modsb"], ["modrow"])

    def norm_setup(l, which, sets):
        ng = norm1_g if which == 1 else norm2_g
        o_sh = 0 if which == 1 else 3
        res = {}
        gb = A.tile([128, D], F32, "gb")
        bcload(gb, ng[l:l + 1, :], D, "gb")
        for s in sets:
            gs = A.tile([128, D], F32, "gs"); sh = A.tile([128, D], F32, "sh")
            bcload(gs, modrow[l, s:s + 1, (o_sh + 1) * D:(o_sh + 2) * D], D, "gs%d" % s, eng="scalar")
            bcload(sh, modrow[l, s:s + 1, o_sh * D:(o_sh + 1) * D], D, "sh%d" % s)
            stt("vector", gs[:], gs[:], 1.0, gb[:], ALU.add, ALU.mult, ["gs%d" % s, "gb"], ["gs%d" % s])
            res[s] = (gs, sh, "gs%d" % s, "sh%d" % s)
        return res

    def norm_alloc():
        return dict(
            xt=[A.tile([128, D], F32, "xt") for _ in range(2)],
            y=[A.tile([128, D], F32, "y") for _ in range(2)],
            nb=[A.tile([128, D], BF16, "nb") for _ in range(2)],
            ss=[A.tile([128, 1], F32, "ss") for _ in range(2)],
            rstd=[A.tile([128, 1], F32, "rstd") for _ in range(2)],
        )

    def norm_tile(nbuf, i, t, gsh, src=None):
        s = i % 2
        xt, y, nb, ss, rstd = nbuf["xt"][s], nbuf["y"][s], nbuf["nb"][s], nbuf["ss"][s], nbuf["rstd"][s]
        gs, sh, kgs, ksh = gsh
        k = "n%d" % s
        srcap = h[t * 128:(t + 1) * 128, :] if src is None else src
        dma("sync" if i % 2 == 0 else "scalar", xt[:], srcap, k + "xt", [("h", t)], [k + "xt"])
        act(nb[:], xt[:], AF.Square, [k + "xt"], [k + "nb", k + "ss"], accum_out=ss[:])
        rstd_from_ss(ss[:], rstd[:], D, k)
        stt("vector", y[:], xt[:], rstd[:, 0:1], gs[:], ALU.mult, ALU.mult, [k + "xt", k + "rstd", kgs], [k + "y"])
        tt("vector", nb[:], y[:], sh[:], ALU.add, [k + "y", ksh], [k + "nb"])
        return s

    def transpose_to(nb_ap, rows, dst_ap, bank, r, w, eng="scalar"):
        pv = psbf(bank)
        for j in range(8):
            tr(pv[:, j, 0:rows], nb_ap[:, j * 128:(j + 1) * 128], ident_bf[0:rows, 0:rows], r + ["identb"], ["ps%d" % bank])
        cp(eng, dst_ap, pv[:, :, 0:rows], ["ps%d" % bank], w)

    def phase_attn(l=0):
        lam_init = 0.8 - 0.6 * math.exp(-0.3 * l)
        A.reset(base_mark)
        QT = A.tile([128, 8, 2304], BF16, "QT"); KT = A.tile([128, 8, 2304], BF16, "KT")
        Vaug = A.tile([128, NT, 8, 129], BF16, "Vaug")
        m1 = A.mark()
        nT = A.tile([128, 8, 2304], BF16, "nT")
        gsh = norm_setup(l, 1, (0, 1))
        nbuf = norm_alloc()
        for i, t in enumerate(range(NT)):
            s = norm_tile(nbuf, i, t, gsh[0 if t < NTL else 1])
            transpose_to(nbuf["nb"][s][:], 128, nT[:, :, t * 128:(t + 1) * 128], 4 + i % 2, ["n%dnb" % s], [("nT", t)],
                         eng="scalar" if i % 2 else "vector")
        P.barrier()
        if attn_stop == "norm":
            return
        A.reset(m1 + 8 * 2304 * 2)
        nT_all = [("nT", t) for t in range(NT)]
        wq = [A.tile([128, 8, D], BF16, "wq") for _ in range(2)]
        ropeC = A.tile([128, 2048], F32, "ropeC"); ropeS = A.tile([128, 2048], F32, "ropeS")
        t1 = [A.tile([128, 512], F32, "t1") for _ in range(2)]
        t2 = [A.tile([128, 512], F32, "t2") for _ in range(2)]
        dma("sync", ropeC[:], rope_c, "ropeC", [], ["ropeC"])
        dma("scalar", ropeS[:], rope_s, "ropeS", [], ["ropeS"])
        P.op("gpsimd", lambda e: e.memset(Vaug[:, :, :, 128:129], 1.0), [], ["Vones"])
        it = 0
        wv5 = [wq[i][:].rearrange("p j (b h f) -> p j b h f", h=2, f=16) for i in range(2)]
        for part, dstT, dk in ((0, QT, "QT"), (1, KT, "KT")):
            cast_rows(wq[0], w_qkv, part * D, D, "wq0")
            for hh in range(2):
                for j in range(8):
                    cp("scalar" if (j + hh) % 2 else "vector", wv5[1][:, j, :, hh, :], wv5[0][:, j, :, 1 - hh, :], ["wq0"], ["wq1"])
            for c in range(8):
                for n in range(5):
                    n0, nw = (n * 512, 512) if n < 4 else (2048, 256)
                    b = it % 2; it += 1
                    for j in range(8):
                        mm(psb[b][:, 0:nw], wq[0][:, j, c * 128:(c + 1) * 128], nT[:, j, n0:n0 + nw], j == 0, j == 7,
                           ["wq0"] + nT_all, ["ps%d" % b])
                    if n == 4:
                        cp("scalar", dstT[:, c, n0:n0 + nw], psb[b][:, 0:nw], ["ps%d" % b], [(dk, c, n)])
                    else:
                        for j in range(8):
                            mm(psb[2 + b][:, :], wq[1][:, j, c * 128:(c + 1) * 128], nT[:, j, n0:n0 + nw], j == 0, j == 7,
                               ["wq1"] + nT_all, ["ps%d" % (2 + b)])
                        tt("vector", t1[b][:], psb[b][:, :], ropeC[:, n0:n0 + 512], ALU.mult, ["ps%d" % b, "ropeC"], ["t1%d" % b])
                        tt("vector", t2[b][:], psb[2 + b][:, :], ropeS[:, n0:n0 + 512], ALU.mult, ["ps%d" % (2 + b), "ropeS"], ["t2%d" % b])
                        tt("vector", dstT[:, c, n0:n0 + 512], t1[b][:], t2[b][:], ALU.add, ["t1%d" % b, "t2%d" % b], [(dk, c, n)])
        if attn_stop in ("proj_norope", "proj_qk"):
            P.barrier()
            return
        cast_rows(wq[0], w_qkv, 2 * D, D, "wq0")
        for t in range(NT):
            for n in range(2):
                b = it % 2; it += 1
                for j in range(8):
                    mm(psb[b][:, :], nT[:, j, t * 128:(t + 1) * 128], wq[0][:, j, n * 512:(n + 1) * 512], j == 0, j == 7,
                       ["wq0"] + nT_all, ["ps%d" % b])
                cp("scalar" if it % 2 else "vector", Vaug[:, t, n * 4:(n + 1) * 4, 0:128],
                   psb[b][:].rearrange("p (a b) -> p a b", a=4), ["ps%d" % b], [("V", t, n)])
        P.barrier()
        if attn_stop == "proj":
            return
        A.reset(m1)
        ON = A.tile([128, NT, D], BF16, "ON")
        m2 = A.mark()
        ET = [A.tile([128, 512], BF16, "ET") for _ in range(3)]
        O1 = A.tile([128, 4, 128], F32, "O1")
        od = [A.tile([128, 128], F32, "od") for _ in range(2)]
        junk = [A.tile([128, 128], BF16, "junk") for _ in range(2)]
        rs = [A.tile([128, 1], F32, "rs") for _ in range(4)]
        rs2 = [A.tile([128, 1], F32, "rs2") for _ in range(2)]
        ss2 = [A.tile([128, 1], F32, "ss2") for _ in range(2)]
        r2 = [A.tile([128, 1], F32, "r2") for _ in range(2)]
        lqk = [A.tile([128, 64], F32, "lqk") for _ in range(4)]
        lpr = A.tile([128, 64], F32, "lpr")
        lsum = A.tile([128, 2], F32, "lsum")
        neglam = A.tile([128, 1], F32, "neglam")
        sg = A.tile([128, 128], F32, "sg")
        for i, src in enumerate((lq1, lk1, lq2, lk2)):
            bcload(lqk[i], src, 64, "lqk%d" % i)
        bcload(sg, subln_g, 128, "sg")
        ts("vector", sg[:], sg[:], 1.0 - lam_init, None, ALU.mult, None, ["sg"], ["sg"])
        for i in range(2):
            tt("vector", lpr[:], lqk[2 * i][:], lqk[2 * i + 1][:], ALU.mult, ["lqk%d" % (2 * i), "lqk%d" % (2 * i + 1)], ["lpr"])
            P.op("vector", lambda e, i=i: e.reduce_sum(out=lsum[:, i:i + 1], in_=lpr[:], axis=AX.X), ["lpr"], ["lsum%d" % i])
        act(lsum[:], lsum[:], AF.Exp, ["lsum0", "lsum1"], ["lsum0", "lsum1"])
        tt("vector", neglam[:], lsum[:, 1:2], lsum[:, 0:1], ALU.subtract, ["lsum0", "lsum1"], ["neglam"])
        ts("vector", neglam[:], neglam[:], -lam_init, None, ALU.add, None, ["neglam"], ["neglam"])
        si = 0; ei = 0; oi = 0
        qchunks = [(n * 512, 512, list(range(NT))) for n in range(4)] + [(2048, 256, [16, 17])]
        for hd in range(8):
            for (q0, qw, ktiles) in qchunks:
                nq = qw // 128
                for comp in range(2):
                    pb = comp * 64
                    def pv(ki, kt, e_):
                        for qs in range(nq):
                            mm(psb[4 + qs][:, 0:129], ET[e_][:, qs * 128:(qs + 1) * 128], Vaug[:, kt, hd, :],
                               ki == 0, ki == len(ktiles) - 1, ["ET%d" % e_], ["ps%d" % (4 + qs)])
                    prev = None
                    for ki, kt in enumerate(ktiles):
                        sbk = si % 2; si += 1
                        mm(psb[sbk][:, 0:qw], KT[pb:pb + 64, hd, kt * 128:(kt + 1) * 128], QT[pb:pb + 64, hd, q0:q0 + qw],
                           True, True, [], ["ps%d" % sbk])
                        e_ = ei % 3; ei += 1
                        act(ET[e_][:, 0:qw], psb[sbk][:, 0:qw], AF.Exp, ["ps%d" % sbk], ["ET%d" % e_], scale=0.125)
                        if prev is not None:
                            pv(*prev)
                        prev = (ki, kt, e_)
                    pv(*prev)
                    for qs in range(nq):
                        qt = q0 // 128 + qs
                        pk = "ps%d" % (4 + qs)
                        P.op("vector", lambda e, qs=qs: e.reciprocal(out=rs[qs][:], in_=psb[4 + qs][:, 128:129]), [pk], ["rs%d" % qs])
                        if comp == 0:
                            ts("vector", O1[:, qs, :], psb[4 + qs][:, 0:128], rs[qs][:, 0:1], None, ALU.mult, None,
                               [pk, "rs%d" % qs], ["O1%d" % qs])
                        else:
                            o = oi % 2; oi += 1
                            tt("vector", rs2[o][:], rs[qs][:], neglam[:], ALU.mult, ["rs%d" % qs, "neglam"], ["rs2%d" % o])
                            stt("vector", od[o][:], psb[4 + qs][:, 0:128], rs2[o][:, 0:1], O1[:, qs, :], ALU.mult, ALU.add,
                                [pk, "rs2%d" % o, "O1%d" % qs], ["od%d" % o])
                            act(junk[o][:], od[o][:], AF.Square, ["od%d" % o], ["junk%d" % o, "a%dss" % o], accum_out=ss2[o][:])
                            ts("vector", r2[o][:], ss2[o][:], 1.0 / 128, EPS, ALU.mult, ALU.add, ["a%dss" % o], ["r2%d" % o])
                            act(r2[o][:], r2[o][:], AF.Sqrt, ["r2%d" % o], ["r2%d" % o])
                            P.op("vector", lambda e, o=o: e.reciprocal(out=r2[o][:], in_=r2[o][:]), ["r2%d" % o], ["r2%d" % o])
                            ts("vector", od[o][:], od[o][:], r2[o][:, 0:1], None, ALU.mult, None, ["od%d" % o, "r2%d" % o], ["od%d" % o])
                            tt("vector", ON[:, qt, hd * 128:(hd + 1) * 128], od[o][:], sg[:], ALU.mult,
                               ["od%d" % o, "sg"], [("ON", qt, hd)])
        P.barrier()
        if attn_stop == "core":
            return
        A.reset(m2)
        wo = A.tile([128, 8, D], BF16, "wo")
        cast_rows(wo, w_o, 0, D, "wo")
        out_proj(lambda t, slot: ON[:, t, :], None, wo, l, list(range(NT)), transpose=True)

    def out_proj(src_fn, zT, wo, l, tiles, transpose):
        g1 = {}
        for s in ((0, 1) if len(tiles) > NTL else (0,)):
            g1[s] = A.tile([128, D], F32, "g1bc")
            bcload(g1[s], modrow[l, s:s + 1, 2 * D:3 * D], D, "g1bc%d" % s)
        ONT = [A.tile([128, 8, 128], BF16, "ONT") for _ in range(2)]
        ht = [A.tile([128, D], F32, "ht") for _ in range(2)]
        tmp = [A.tile([128, D], F32, "tmp") for _ in range(2)]
        it = 0
        for i, t in enumerate(tiles):
            s = i % 2
            st = 0 if t < NTL else 1
            dma("sync" if s == 0 else "scalar", ht[s][:], h[t * 128:(t + 1) * 128, :], "ht%d" % s, [], ["ht%d" % s])
            if transpose:
                transpose_to(src_fn(t, s), 128, ONT[s][:], 2 + s, [], ["ONT%d" % s])
            for n in range(2):
                b = it % 2; it += 1
                for j in range(8):
                    lhs = ONT[s][:, j, :] if transpose else zT[:, j, t * 128:(t + 1) * 128]
                    mm(psb[b][:, :], lhs, wo[:, j, n * 512:(n + 1) * 512], j == 0, j == 7,
                       ["wo"] + (["ONT%d" % s] if transpose else []), ["ps%d" % b])
                tt("vector", tmp[s][:, n * 512:(n + 1) * 512], psb[b][:, :], g1[st][:, n * 512:(n + 1) * 512], ALU.mult,
                   ["ps%d" % b, "g1bc%d" % st], ["tmp%d%d" % (s, n)])
                tt("vector", ht[s][:, n * 512:(n + 1) * 512], ht[s][:, n * 512:(n + 1) * 512], tmp[s][:, n * 512:(n + 1) * 512], ALU.add,
                   ["tmp%d%d" % (s, n), "ht%d" % s], ["ht%d" % s])
            dma("sync" if s == 0 else "scalar", h[t * 128:(t + 1) * 128, :], ht[s][:], "ht%d" % s, ["ht%d" % s], [])

    def phase_moe(l, with_ctx):
        A.reset(base_mark)
        sets = (0, 1) if with_ctx else (0,)
        tiles = list(range(NT if with_ctx else NTL))
        ncap = [256, 32]
        probsT = A.tile([16, 2304], F32, "probsT")
        wr = A.tile([128, 8, NE], BF16, "wr")
        cast_rows(wr, w_router[l], 0, NE, "wr")
        idxT = A.tile([128, 3, NE], I32, "idxT"); gateT = A.tile([128, 3, NE], F32, "gateT")
        g2 = {}
        for s in sets:
            g2[s] = A.tile([128, D], F32, "g2bc")
            bcload(g2[s], modrow[l, s:s + 1, 5 * D:6 * D], D, "g2bc%d" % s)
        m0 = A.mark()
        gsh = norm_setup(l, 2, sets)
        nbuf = norm_alloc()
        nTt = [A.tile([128, 8, 128], BF16, "nTt") for _ in range(2)]
        lg = [A.tile([128, NE], F32, "lg") for _ in range(2)]
        mxl = [A.tile([128, 1], F32, "mxl") for _ in range(2)]
        sme = [A.tile([128, 1], F32, "sme") for _ in range(2)]
        for i, t in enumerate(tiles):
            s = norm_tile(nbuf, i, t, gsh[0 if t < NTL else 1])
            k = "r%d" % s
            dma("sync", mbf[t * 128:(t + 1) * 128, :], nbuf["nb"][s][:], "n%dnb" % s, ["n%dnb" % s], [("mbf", t)])
            transpose_to(nbuf["nb"][s][:], 128, nTt[s][:], 4 + s, ["n%dnb" % s], [k + "nTt"])
            for j in range(8):
                mm(psb[6 + s][:, 0:NE], nTt[s][:, j, :], wr[:, j, :], j == 0, j == 7, [k + "nTt", "wr"], ["ps%d" % (6 + s)])
            P.op("vector", lambda e, s=s: e.reduce_max(out=mxl[s][:], in_=psb[6 + s][:, 0:NE], axis=AX.X), ["ps%d" % (6 + s)], [k + "mx"])
            ts("vector", mxl[s][:], mxl[s][:], -1.0, None, ALU.mult, None, [k + "mx"], [k + "mx"])
            act(lg[s][:], psb[6 + s][:, 0:NE], AF.Exp, ["ps%d" % (6 + s), k + "mx"], [k + "lg", k + "sm"], bias=mxl[s][:, 0:1], accum_out=sme[s][:])
            P.op("vector", lambda e, s=s: e.reciprocal(out=sme[s][:], in_=sme[s][:]), [k + "sm"], [k + "sm"])
            ts("vector", lg[s][:], lg[s][:], sme[s][:, 0:1], None, ALU.mult, None, [k + "lg", k + "sm"], [k + "lg"])
            tr(psb[s][0:NE, 0:128], lg[s][:], ident_f[:], [k + "lg", "identf"], ["ps%d" % s])
            cp("vector", probsT[:, t * 128:(t + 1) * 128], psb[s][0:NE, 0:128], ["ps%d" % s], [("pT", t)])
        P.barrier()
        A.reset(m0)
        work = A.tile([16, 2048], F32, "work")
        gate = A.tile([16, 288], F32, "gate"); idxu = A.tile([16, 288], U32, "idxu"); idxf = A.tile([16, 288], F32, "idxf")
        for s in sets:
            n0, ntok, cap, c0 = (0, 2048, 256, 0) if s == 0 else (2048, 256, 32, 256)
            cp("vector", work[:, 0:ntok], probsT[:, n0:n0 + ntok], [], ["work"])
            for r in range(cap // 8):
                g_ = gate[:, c0 + r * 8:c0 + (r + 1) * 8]
                P.op("vector", lambda e, g_=g_, ntok=ntok: e.max(out=g_, in_=work[:, 0:ntok]), ["work"], ["gate"])
                P.op("vector", lambda e, g_=g_, ntok=ntok, r=r, c0=c0: e.max_index(out=idxu[:, c0 + r * 8:c0 + (r + 1) * 8], in_max=g_, in_values=work[:, 0:ntok]),
                     ["work", "gate"], ["idxu"])
                P.op("vector", lambda e, g_=g_, ntok=ntok: e.match_replace(out=work[:, 0:ntok], in_to_replace=g_, in_values=work[:, 0:ntok], imm_value=-1.0),
                     ["work", "gate"], ["work"])
        ncol = 288 if with_ctx else 256
        cp("vector", idxf[:, 0:ncol], idxu[:, 0:ncol], ["idxu"], ["idxf"])
        if with_ctx:
            ts("vector", idxf[:, 256:288], idxf[:, 256:288], 2048.0, None, ALU.add, None, ["idxf"], ["idxf"])
        parts = [(0, 0, 128), (1, 128, 128)] + ([(2, 256, 32)] if with_ctx else [])
        for (pi, c0, rows) in parts:
            tr(psb[0][0:rows, 0:NE], idxf[:, c0:c0 + rows], ident_f[0:NE, 0:NE], ["idxf", "identf"], ["ps0"])
            cp("vector", idxT[0:rows, pi, :], psb[0][0:rows, 0:NE], ["ps0"], ["idxT"])
            tr(psb[1][0:rows, 0:NE], gate[:, c0:c0 + rows], ident_f[0:NE, 0:NE], ["gate", "identf"], ["ps1"])
            cp("vector", gateT[0:rows, pi, :], psb[1][0:rows, 0:NE], ["ps1"], ["gateT"])
        P.barrier()
        A.reset(m0)
        ntk = 288 if with_ctx else 256
        Xg = [[A.tile([128, D], BF16, "Xg") for _ in parts] for _ in range(2)]
        XgT = [A.tile([128, 8, ntk], BF16, "XgT") for _ in range(2)]
        NWG = 4
        wg = [A.tile([128, 8, 1024], BF16, "wg") for _ in range(NWG)]
        wd = [A.tile([128, 2, D], BF16, "wd") for _ in range(NWG)]
        hT = A.tile([128, 22, ntk], BF16, "hT")
        sgt = [A.tile([128, ntk], F32, "sgt") for _ in range(2)]
        ysb = [A.tile([128, D], F32, "ysb") for _ in range(len(parts) * 2)]

        def gather(e):
            sl = e % 2
            for (pi, c0, rows) in parts:
                P.dma("gpsimd", lambda en, pi=pi, rows=rows, sl=sl, e=e: en.indirect_dma_start(
                    out=Xg[sl][pi][0:rows, :], out_offset=None, in_=mbf[:, :],
                    in_offset=bass.IndirectOffsetOnAxis(ap=idxT[0:rows, pi, e:e + 1], axis=0)),
                    "Xg%d%d" % (sl, pi), [], ["Xg%d%d" % (sl, pi)])

        def xpose(e):
            sl = e % 2
            for (pi, c0, rows) in parts:
                transpose_to(Xg[sl][pi][0:rows, :], rows, XgT[sl][:, :, c0:c0 + rows], 2 + pi % 2,
                             ["Xg%d%d" % (sl, pi)], [("XgT", sl, pi)], eng="vector")

        wgi = [0]; wdi = [0]; gi = [0]; yi = [0]

        def load_wg(e, b):
            s = wgi[0] % NWG; wgi[0] += 1
            nf = 512 if b < 5 else 256
            cast_rows(wg[s], w_gu[l, e], b * 512, nf, "wg%d" % s, 0)
            cast_rows(wg[s], w_gu[l, e], FE + b * 512, nf, "wg%d" % s, 512)
            return s

        def load_wd(e, b):
            s = wdi[0] % NWG; wdi[0] += 1
            cast_rows(wd[s], w_dn[l, e][2 * b * 128:(2 * b + 2) * 128, :], 0, D, "wd%d" % s)
            return s

        blocks = []
        for e in range(NE):
            blocks += [("g", e, b) for b in range(6)] + [("d", e, b) for b in range(11)]
        PF = 3
        slots = {}
        gather(0)
        xpose(0)
        for bi in range(min(PF, len(blocks))):
            kind, e, b = blocks[bi]
            slots[bi] = load_wg(e, b) if kind == "g" else load_wd(e, b)
        for bi, (kind, e, b) in enumerate(blocks):
            nb_ = bi + PF
            if nb_ < len(blocks):
                k2, e2, b2 = blocks[nb_]
                slots[nb_] = load_wg(e2, b2) if k2 == "g" else load_wd(e2, b2)
            sl = e % 2
            xk = [("XgT", sl, pi) for (pi, _, _) in parts]
            if kind == "g":
                if b == 0 and e + 1 < NE:
                    gather(e + 1)
                s = slots[bi]
                for sub in range(4 if b < 5 else 2):
                    c = b * 4 + sub
                    gb = gi[0] % 2; gi[0] += 1
                    for gu, colb in ((0, sub * 128), (1, 512 + sub * 128)):
                        bank = gb * 2 + gu
                        for j in range(8):
                            mm(psb[bank][:, 0:ntk], wg[s][:, j, colb:colb + 128], XgT[sl][:, j, :], j == 0, j == 7,
                               ["wg%d" % s] + xk, ["ps%d" % bank])
                    act(sgt[gb][:], psb[gb * 2][:, 0:ntk], AF.Silu, ["ps%d" % (gb * 2)], ["sgt%d" % gb])
                    tt("vector", hT[:, c, :], sgt[gb][:], psb[gb * 2 + 1][:, 0:ntk], ALU.mult,
                       ["sgt%d" % gb, "ps%d" % (gb * 2 + 1)], [("hT", c)])
            else:
                s = slots[bi]
                if b == 0 and e + 1 < NE:
                    xpose(e + 1)
                for cc in range(2):
                    c = 2 * b + cc
                    for (pi, c0, rows) in parts:
                        for dh in range(2):
                            bank = (4 + pi * 2 + dh) if pi < 2 else dh
                            mm(psb[bank][0:rows, :], hT[:, c, c0:c0 + rows], wd[s][:, cc, dh * 512:(dh + 1) * 512],
                               c == 0, c == 21, ["wd%d" % s, ("hT", c)], ["ps%d" % bank])
                if b == 10:
                    for (pi, c0, rows) in parts:
                        yb = yi[0] % len(ysb); yi[0] += 1
                        st = 0 if pi < 2 else 1
                        for dh in range(2):
                            bank = (4 + pi * 2 + dh) if pi < 2 else dh
                            stt("vector", ysb[yb][0:rows, dh * 512:(dh + 1) * 512], psb[bank][0:rows, :], gateT[0:rows, pi, e:e + 1],
                                g2[st][0:rows, dh * 512:(dh + 1) * 512], ALU.mult, ALU.mult,
                                ["ps%d" % bank, "gateT", "g2bc%d" % st], ["ysb%d" % yb])
                        P.dma("gpsimd", lambda en, pi=pi, rows=rows, yb=yb, e=e: en.indirect_dma_start(
                            out=h[:, :], out_offset=bass.IndirectOffsetOnAxis(ap=idxT[0:rows, pi, e:e + 1], axis=0),
                            in_=ysb[yb][0:rows, :], in_offset=None, compute_op=ALU.add),
                            "ysb%d" % yb, ["ysb%d" % yb] + [("hsc", e - 1, p2) for (p2, _, _) in parts], [("hsc", e, pi)])

    def phase_lru(l=1):
        A.reset(base_mark)
        nT = A.tile([128, 8, 2304], BF16, "nT")
        zT = A.tile([128, 8, 2048], BF16, "zT")
        m1 = A.mark()
        gsh = norm_setup(l, 1, (0, 1))
        nbuf = norm_alloc()
        for i, t in enumerate(range(NT)):
            s = norm_tile(nbuf, i, t, gsh[0 if t < NTL else 1])
            transpose_to(nbuf["nb"][s][:], 128, nT[:, :, t * 128:(t + 1) * 128], 4 + i % 2, ["n%dnb" % s], [("nT", t)],
                         eng="scalar" if i % 2 else "vector")
        P.barrier()
        A.reset(m1)
        nT_all = []
        win = A.tile([128, 8, 2 * D], BF16, "win")
        cast_rows(win, lru_w_in, 0, 2 * D, "win")
        wgt = A.tile([128, 2, 8, 256], BF16, "wgt")
        for d_ in range(2):
            for k_ in range(8):
                dma("gpsimd", wgt[:, d_, k_, :], w_gates[d_, k_], "wgt", [], ["wgt"])
        bin_ = A.tile([128, 16], F32, "bin")
        dma("sync", bin_[:], lru_b_in.rearrange("(j p) -> p j", p=128), "bin", [], ["bin"], allow_slow_non_contiguous=True)
        cw = A.tile([128, 4, 8], F32, "cw")
        dma("sync", cw[:], conv_w.rearrange("j (k p) -> p j k", p=128), "cw", [], ["cw"], allow_slow_non_contiguous=True)
        cb = A.tile([128, 8], F32, "cb")
        dma("sync", cb[:], conv_b.rearrange("(k p) -> p k", p=128), "cb", [], ["cb"], allow_slow_non_contiguous=True)
        bg = A.tile([128, 2, 8, 2], F32, "bg")
        dma("sync", bg[:], b_gates.rearrange("d k (hf p) -> p d k hf", p=128), "bg", [], ["bg"], allow_slow_non_contiguous=True)
        lamt = A.tile([128, 2, 8], F32, "lamt")
        dma("sync", lamt[:], lru_lam.rearrange("d (k p) -> p d k", p=128), "lamt", [], ["lamt"], allow_slow_non_contiguous=True)
        sdec = A.tile([128, 2, 8], F32, "sdec"); sdec2 = A.tile([128, 2, 8], F32, "sdec2")
        act(sdec[:], lamt[:], AF.Exp, ["lamt"], ["sdec"], scale=-1.0)
        act(sdec[:], sdec[:], AF.Ln, ["sdec"], ["sdec"], bias=1.0)
        ts("vector", sdec2[:], sdec[:], -16.0, None, ALU.mult, None, ["sdec"], ["sdec2"])
        ts("vector", sdec[:], sdec[:], -8.0, None, ALU.mult, None, ["sdec", "sdec2"], ["sdec"])
        LC, LL = 256, 2048
        xpc = A.tile([128, LC + 3], F32, "xpc"); xpl = A.tile([128, LL + 3], F32, "xpl")
        xc = A.tile([128, 2304], F32, "xc")
        xb = A.tile([128, 2304], BF16, "xb")
        yg = A.tile([128, 2048], F32, "yg")
        rg = A.tile([128, 2304], F32, "rg"); ig = A.tile([128, 2304], F32, "ig")
        at = A.tile([128, 2304], F32, "at"); qt_ = A.tile([128, 2304], F32, "qt")
        hf = A.tile([128, 2304], F32, "hf"); hb = A.tile([128, 2304], F32, "hb")
        for tl in (xpc, xpl):
            P.op("gpsimd", lambda e, tl=tl: e.memset(tl[:], 0.0), [], ["xp"])
        it = 0
        chunks = [(n * 512, 512) for n in range(4)] + [(2048, 256)]
        for k in range(8):
            for (n0, nw) in chunks:
                b = it % 2; it += 1
                for j in range(8):
                    mm(psb[b][:, 0:nw], win[:, j, D + k * 128:D + (k + 1) * 128], nT[:, j, n0:n0 + nw], j == 0, j == 7, ["win"], ["ps%d" % b])
                dst = xpl[:, 2 + n0:2 + n0 + nw] if n0 < 2048 else xpc[:, 2:2 + LC]
                act(dst, psb[b][:, 0:nw], AF.Identity, ["ps%d" % b, "bin"], ["xp"], bias=bin_[:, 8 + k:9 + k])
                if n0 < 2048:
                    b = it % 2; it += 1
                    for j in range(8):
                        mm(psb[b][:, 0:nw], win[:, j, k * 128:(k + 1) * 128], nT[:, j, n0:n0 + nw], j == 0, j == 7, ["win"], ["ps%d" % b])
                    act(yg[:, n0:n0 + nw], psb[b][:, 0:nw], AF.Gelu_apprx_tanh, ["ps%d" % b, "bin"], ["yg"], bias=bin_[:, k:k + 1])
            for (xp, o0, L) in ((xpc, 0, LC), (xpl, LC, LL)):
                ts("vector", xc[:, o0:o0 + L], xp[:, 0:L], cw[:, 0, k:k + 1], cb[:, k:k + 1], ALU.mult, ALU.add, ["xp", "cw", "cb"], ["xc"])
                for j in range(1, 4):
                    stt("vector", xc[:, o0:o0 + L], xp[:, j:j + L], cw[:, j, k:k + 1], xc[:, o0:o0 + L], ALU.mult, ALU.add, ["xp", "cw", "xc"], ["xc"])
            cp("vector", xb[:], xc[:], ["xc"], ["xb"])
            for d in range(2):
                for half, dstt, dk in ((0, rg, "rg"), (1, ig, "ig")):
                    for (n0, nw) in [(0, 256)] + [(256 + n * 512, 512) for n in range(4)]:
                        b = it % 2; it += 1
                        mm(psb[b][:, 0:nw], wgt[:, d, k, half * 128:(half + 1) * 128], xb[:, n0:n0 + nw], True, True, ["wgt", "xb"], ["ps%d" % b])
                        act(dstt[:, n0:n0 + nw], psb[b][:, 0:nw], AF.Sigmoid, ["ps%d" % b, "bg"], [dk], bias=bg[:, d, k, half:half + 1])
                act(at[:], rg[:], AF.Exp, ["rg", "sdec"], ["at"], scale=sdec[:, d, k:k + 1])
                act(qt_[:], rg[:], AF.Exp, ["rg", "sdec2"], ["qt"], scale=sdec2[:, d, k:k + 1])
                act(qt_[:], qt_[:], AF.Sqrt, ["qt"], ["qt"], scale=-1.0, bias=1.0)
                tt("vector", ig[:], ig[:], xc[:], ALU.mult, ["ig", "xc"], ["ig"])
                tt("vector", ig[:], ig[:], qt_[:], ALU.mult, ["ig", "qt"], ["ig"])
                hh, hk = (hf, "hf") if d == 0 else (hb, "hb")
                if d == 0:
                    P.op("vector", lambda e, hh=hh: e.tensor_tensor_scan(out=hh[:, 0:LC], data0=at[:, 0:LC], data1=ig[:, 0:LC], initial=0.0,
                                                                     op0=ALU.mult, op1=ALU.add), ["at", "ig"], [hk])
                    P.op("vector", lambda e, hh=hh: e.tensor_tensor_scan(out=hh[:, LC:], data0=at[:, LC:], data1=ig[:, LC:], initial=hh[:, LC - 1:LC],
                                                                     op0=ALU.mult, op1=ALU.add), ["at", "ig", hk], [hk])
                else:
                    P.op("vector", lambda e, hh=hh: e.tensor_tensor_scan(out=hh[:, 0:LC][:, ::-1], data0=at[:, 0:LC][:, ::-1], data1=ig[:, 0:LC][:, ::-1],
                                                                     initial=0.0, op0=ALU.mult, op1=ALU.add), ["at", "ig"], [hk])
                    P.op("vector", lambda e, hh=hh: e.tensor_tensor_scan(out=hh[:, LC:][:, ::-1], data0=at[:, LC:][:, ::-1], data1=ig[:, LC:][:, ::-1],
                                                                     initial=hh[:, 0:1], op0=ALU.mult, op1=ALU.add), ["at", "ig", hk], [hk])
            tt("vector", hf[:, LC:], hf[:, LC:], hb[:, LC:], ALU.add, ["hf", "hb"], ["hf"])
            tt("vector", zT[:, k, :], hf[:, LC:], yg[:], ALU.mult, ["hf", "yg"], [("zT", k)])
        P.barrier()
        A.reset(m1)
        wo = A.tile([128, 8, D], BF16, "wo")
        cast_rows(wo, lru_w_out, 0, D, "wo")
        out_proj(None, zT, wo, l, list(range(NTL)), transpose=False)

    def phase_final():
        A.reset(base_mark)
        fg = A.tile([128, D], F32, "fg")
        bcload(fg, final_g, D, "fg")
        xt = [A.tile([128, D], F32, "fxt") for _ in range(2)]
        yo = [A.tile([128, D], F32, "fyo") for _ in range(2)]
        jk = [A.tile([128, D], BF16, "fjk") for _ in range(2)]
        ss = [A.tile([128, 1], F32, "fss") for _ in range(2)]
        rstd = [A.tile([128, 1], F32, "frs") for _ in range(2)]
        for t in range(NTL):
            s = t % 2; k = "f%d" % s
            dma("sync" if s == 0 else "scalar", xt[s][:], h[t * 128:(t + 1) * 128, :], k + "xt", [], [k + "xt"])
            act(jk[s][:], xt[s][:], AF.Square, [k + "xt"], [k + "jk", k + "ss"], accum_out=ss[s][:])
            rstd_from_ss(ss[s][:], rstd[s][:], D, k)
            stt("vector", yo[s][:], xt[s][:], rstd[s][:, 0:1], fg[:], ALU.mult, ALU.mult, [k + "xt", k + "rstd", "fg"], [k + "yo"])
            dma("sync" if s == 0 else "scalar", out[t * 128:(t + 1) * 128, :], yo[s][:], k + "yo", [k + "yo"], [])

    dma("sync", h[0:2048, :], x_in, "hinit0", [], [])
    dma("scalar", h[2048:2304, :], ctx_in, "hinit1", [], [])
    stages = ["mod", "attn", "moe0", "lru", "moe1", "final"]
    nst = len(stages) if stop_after is None else stages.index(stop_after) + 1
    for st in stages[:nst]:
        if st == "mod":
            phase_mod()
        elif st == "attn":
            phase_attn(0)
        elif st == "moe0":
            phase_moe(0, True)
        elif st == "lru":
            phase_lru(1)
        elif st == "moe1":
            phase_moe(1, False)
        elif st == "final":
            phase_final()
        P.barrier()
    P.wait_all("sync")
    P.emit()
    return nc


_CONST = {}


def make_in_maps(inputs):
    if "C" not in _CONST:
        C, S, perm = rope_tables()
        _CONST.update(C=C, S=S, perm=perm, ident=np.eye(128, dtype=np.float32))
    g = lambda k: np.ascontiguousarray(inputs[k])
    shared = {
        "c_ctx": g("c_ctx"), "ada_w": g("ada_w"), "ada_b": g("ada_b"), "norm1_g": g("norm1_g"), "norm2_g": g("norm2_g"),
        "final_g": g("final_g").reshape(1, D), "attn_w_qkv": g("attn_w_qkv")[0],
        "attn_lq1": g("attn_lq1"), "attn_lk1": g("attn_lk1"), "attn_lq2": g("attn_lq2"), "attn_lk2": g("attn_lk2"),
        "attn_subln_g": g("attn_subln_g"), "attn_w_o": g("attn_w_o")[0],
        "lru_w_in": g("lru_w_in")[0], "lru_b_in": g("lru_b_in")[0], "lru_conv_w": g("lru_conv_w")[0], "lru_conv_b": g("lru_conv_b")[0],
        "lru_w_gates": g("lru_w_gates")[0], "lru_b_gates": g("lru_b_gates")[0], "lru_lambda": g("lru_lambda")[0],
        "lru_w_out": g("lru_w_out")[0], "moe_w_router": g("moe_w_router"), "moe_w_gate_up": g("moe_w_gate_up"),
        "moe_w_down": g("moe_w_down"),
        "rope_c": _CONST["C"], "rope_s": _CONST["S"], "perm": _CONST["perm"], "ident": _CONST["ident"],
    }
    maps = []
    for b in range(8):
        m = dict(shared)
        m["x"] = g("x")[b]; m["ctx"] = g("ctx")[b]; m["c"] = g("c")[b]
        maps.append(m)
    return maps


def kernel(**inputs):
    nc = build_program()
    in_maps = make_in_maps(inputs)
    res = run_bass_kernel_spmd(nc, in_maps, core_ids=list(range(8)))
    return np.stack([np.asarray(r["out"]) for r in res.results], axis=0).astype(np.float32)
```

```python
import math
import numpy as np
import ml_dtypes
import concourse.bass as bass
import concourse.mybir as mybir
from concourse.bass_utils import run_bass_kernel_spmd

F32 = mybir.dt.float32
BF16 = mybir.dt.bfloat16
I32 = mybir.dt.int32
U32 = mybir.dt.uint32
AF = mybir.ActivationFunctionType
ALU = mybir.AluOpType
AX = mybir.AxisListType

ENGS = ["sync", "scalar", "vector", "gpsimd", "tensor"]
D = 1024
NTL, NTC, NT = 16, 2, 18
EPS = 1e-6
NE = 16
FE = 2816
DT_SIZE = {F32: 4, BF16: 2, I32: 4, U32: 4}


class Prog:
    def __init__(self, nc, same_engine_sync=("vector", "scalar", "gpsimd")):
        self.nc = nc
        self.stream = {e: [] for e in ENGS}
        self.ecount = {e: 0 for e in ENGS}
        self.chan = {}
        self.last_w = {}
        self.readers = {}
        self.waited = {e: {} for e in ENGS}
        self.same_engine_sync = set(same_engine_sync)
        self.sems = {}

    def _deps(self, reads, writes):
        toks = []
        for r in reads:
            t = self.last_w.get(r)
            if t is not None:
                toks.append(t)
        for w in writes:
            t = self.last_w.get(w)
            if t is not None:
                toks.append(t)
            toks.extend(self.readers.get(w, []))
        return toks

    def _record(self, tok, reads, writes):
        for r in reads:
            self.readers.setdefault(r, []).append(tok)
        for w in writes:
            self.last_w[w] = tok
            self.readers[w] = []

    def _emit_waits(self, eng, toks, all_same=False):
        need = {}
        for (k, v) in toks:
            if k == ("e", eng) and not all_same and eng not in self.same_engine_sync:
                continue
            if v > need.get(k, 0):
                need[k] = v
        for k, v in need.items():
            if self.waited[eng].get(k, 0) >= v:
                continue
            self.waited[eng][k] = v
            self.stream[eng].append(("wait", k, v))

    def op(self, eng, fn, reads=(), writes=()):
        reads = list(reads); writes = list(writes)
        self._emit_waits(eng, self._deps(reads, writes))
        self.ecount[eng] += 1
        tok = (("e", eng), self.ecount[eng])
        self.stream[eng].append(("ins", fn, ("e", eng), 1))
        self._record(tok, reads, writes)
        return tok

    def dma(self, eng, fn, chan, reads=(), writes=()):
        reads = list(reads); writes = list(writes)
        self._emit_waits(eng, self._deps(reads, writes))
        c = self.chan.setdefault(chan, [("c", chan), 0])
        c[1] += 16
        tok = (c[0], c[1])
        self.stream[eng].append(("ins", fn, c[0], 16))
        self._record(tok, reads, writes)
        return tok

    def _all_toks(self):
        toks = [(("e", e), self.ecount[e]) for e in ENGS if self.ecount[e] > 0]
        toks += [(c[0], c[1]) for c in self.chan.values()]
        return toks

    def barrier(self):
        toks = self._all_toks()
        for e in ENGS:
            self._emit_waits(e, toks, all_same=True)
        self.last_w = {}
        self.readers = {}

    def wait_all(self, eng):
        self._emit_waits(eng, self._all_toks(), all_same=True)

    def emit(self):
        nc = self.nc
        keys = [("e", e) for e in ENGS if self.ecount[e] > 0] + [c[0] for c in self.chan.values()]
        for i, k in enumerate(keys):
            self.sems[k] = nc.alloc_semaphore(name="sm%d" % i)
        sems = self.sems
        stream = self.stream

        def run(eng_name):
            def f(eng):
                for item in stream[eng_name]:
                    if item[0] == "wait":
                        eng.wait_ge(sems[item[1]], item[2])
                    else:
                        _, fn, k, inc = item
                        fn(eng).then_inc(sems[k], inc)
            return f

        with nc.Block() as block:
            block.sync(run("sync"))
            block.scalar(run("scalar"))
            block.vector(run("vector"))
            block.gpsimd(run("gpsimd"))
            block.tensor(run("tensor"))


class Arena:
    def __init__(self, nc, base=16512, top=229344):
        self.nc = nc; self.base = base; self.top = top; self.off = base; self.cnt = 0

    def mark(self):
        return self.off

    def reset(self, to=None):
        self.off = self.base if to is None else to

    def tile(self, shape, dt, name="t"):
        n = 1
        for s in shape[1:]:
            n *= s
        nbytes = (n * DT_SIZE[dt] + 31) // 32 * 32
        assert self.off + nbytes <= self.top, ("SBUF overflow", name, self.off, nbytes)
        self.cnt += 1
        t = self.nc.alloc_sbuf_tensor_at("%s_%d" % (name, self.cnt), list(shape), dt, offset=self.off)
        self.off += nbytes
        return t


def rope_tables():
    n = 2048
    row = np.repeat(np.arange(n // 64), 64).astype(np.float32)
    col = np.tile(np.arange(64), n // 64).astype(np.float32)
    inv = (np.float32(10000.0) ** (-(np.arange(16, dtype=np.float32) * np.float32(2.0)) / np.float32(32))).astype(np.float32)
    ang = np.concatenate([row[:, None] * inv, col[:, None] * inv], axis=-1).astype(np.float32)
    cos = np.cos(ang).astype(np.float32); sin = np.sin(ang).astype(np.float32)
    C = np.zeros((128, n), np.float32); S = np.zeros((128, n), np.float32)
    perm = np.zeros((128, 128), np.float32)
    for p in range(128):
        d = p % 64
        a = d // 32; half = (d % 32) // 16; f = d % 16
        C[p] = cos[:, a * 16 + f]
        S[p] = sin[:, a * 16 + f] * (-1.0 if half == 0 else 1.0)
        partner = p + 16 if half == 0 else p - 16
        perm[partner, p] = 1.0
    return C, S, perm


def build_program(debug=False, stop_after=None, attn_stop=None):
    nc = bass.Bass("TRN2", target_bir_lowering=False)
    P = Prog(nc)
    A = Arena(nc)

    def din(name, shape, dt=F32):
        return nc.dram_tensor(name, list(shape), dt, kind="ExternalInput").ap()

    x_in = din("x", [2048, D]); ctx_in = din("ctx", [256, D]); c_in = din("c", [D]); cctx_in = din("c_ctx", [D])
    ada_w = din("ada_w", [2, D, 6 * D]); ada_b = din("ada_b", [2, 6 * D])
    norm1_g = din("norm1_g", [2, D]); norm2_g = din("norm2_g", [2, D]); final_g = din("final_g", [1, D])
    w_qkv = din("attn_w_qkv", [D, 3 * D])
    lq1 = din("attn_lq1", [1, 64]); lk1 = din("attn_lk1", [1, 64]); lq2 = din("attn_lq2", [1, 64]); lk2 = din("attn_lk2", [1, 64])
    subln_g = din("attn_subln_g", [1, 128]); w_o = din("attn_w_o", [D, D])
    lru_w_in = din("lru_w_in", [D, 2 * D]); lru_b_in = din("lru_b_in", [2 * D])
    conv_w = din("lru_conv_w", [4, D]); conv_b = din("lru_conv_b", [D])
    w_gates = din("lru_w_gates", [2, 8, 128, 256]); b_gates = din("lru_b_gates", [2, 8, 256])
    lru_lam = din("lru_lambda", [2, D]); lru_w_out = din("lru_w_out", [D, D])
    w_router = din("moe_w_router", [2, D, NE]); w_gu = din("moe_w_gate_up", [2, NE, D, 2 * FE]); w_dn = din("moe_w_down", [2, NE, FE, D])
    rope_c = din("rope_c", [128, 2048]); rope_s = din("rope_s", [128, 2048]); perm_in = din("perm", [128, 128])
    ident_in = din("ident", [128, 128])
    out = nc.dram_tensor("out", [2048, D], F32, kind="ExternalOutput").ap()
    h = nc.dram_tensor("h_scr", [2304, D], F32, kind="ExternalOutput" if debug else "Internal").ap()
    modrow = nc.dram_tensor("modrow", [2, 2, 6 * D], F32, kind="ExternalOutput" if debug else "Internal").ap()
    mbf = nc.dram_tensor("mbf", [2304, D], BF16).ap()

    psb = [nc.alloc_psum_tensor("psb%d" % i, [128, 512], F32) for i in range(8)]

    def psbf(i):
        return psb[i][:].bitcast(BF16).rearrange("p (j n) -> p j n", j=8)

    def mm(o, lhsT, rhs, start, stop, r, w):
        P.op("tensor", lambda e: e.matmul(o, lhsT=lhsT, rhs=rhs, start=start, stop=stop), r, w)

    def tr(o, i, idn, r, w):
        P.op("tensor", lambda e: e.transpose(o, i, idn), r, w)

    def act(o, i, func, r, w, **kw):
        P.op("scalar", lambda e: e.activation(out=o, in_=i, func=func, **kw), r, w)

    def tt(eng, o, a, b, op, r, w):
        P.op(eng, lambda e: e.tensor_tensor(out=o, in0=a, in1=b, op=op), r, w)

    def ts(eng, o, a, s1, s2, op0, op1, r, w):
        if s2 is None:
            P.op(eng, lambda e: e.tensor_scalar(out=o, in0=a, scalar1=s1, scalar2=None, op0=op0), r, w)
        else:
            P.op(eng, lambda e: e.tensor_scalar(out=o, in0=a, scalar1=s1, scalar2=s2, op0=op0, op1=op1), r, w)

    def stt(eng, o, a, s, b, op0, op1, r, w):
        P.op(eng, lambda e: e.scalar_tensor_tensor(out=o, in0=a, scalar=s, in1=b, op0=op0, op1=op1), r, w)

    def cp(eng, o, i, r, w):
        if eng == "scalar":
            P.op(eng, lambda e: e.copy(out=o, in_=i), r, w)
        else:
            P.op(eng, lambda e: e.tensor_copy(out=o, in_=i), r, w)

    def dma(eng, o, i, chan, r, w, **kw):
        return P.dma(eng, lambda e: e.dma_start(out=o, in_=i, **kw), chan, r, w)

    def cast_finalize(chan, keys):
        c = P.chan[chan]
        for k_ in keys:
            P.last_w[k_] = (c[0], c[1])

    def cast_rows(dst3, src2, col0, ncols, chan, dcol0=0, defer=None):
        J = src2.shape[0] // 128
        keys = [] if defer is None else defer
        for j in range(J):
            for c in range(0, ncols, 1024):
                w_ = min(1024, ncols - c)
                dma("gpsimd", dst3[:, j, dcol0 + c:dcol0 + c + w_], src2[j * 128:(j + 1) * 128, col0 + c:col0 + c + w_], chan, [], [(chan, j, dcol0 + c)])
                keys.append((chan, j, dcol0 + c))
        if defer is None:
            cast_finalize(chan, keys)

    def bcload(tile_, row_ap, n, chan, eng="sync"):
        dma(eng, tile_[:], row_ap.to_broadcast([128, n]), chan, [], [chan])

    ident_bf = A.tile([128, 128], BF16, "identb")
    ident_f = A.tile([128, 128], F32, "identf")
    dma("gpsimd", ident_bf[:], ident_in, "identb", [], ["identb"])
    dma("sync", ident_f[:], ident_in, "identf", [], ["identf"])
    base_mark = A.mark()

    def rstd_from_ss(ss, rstd, n, key):
        ts("vector", rstd, ss, 1.0 / n, EPS, ALU.mult, ALU.add, [key + "ss"], [key + "rstd"])
        act(rstd, rstd, AF.Sqrt, [key + "rstd"], [key + "rstd"])
        P.op("vector", lambda e: e.reciprocal(out=rstd, in_=rstd), [key + "rstd"], [key + "rstd"])

    def phase_mod():
        A.reset(base_mark)
        cl = A.tile([128, 8], F32, "cl"); cc = A.tile([128, 8], F32, "cc")
        sct = A.tile([128, 8, 2], F32, "sct")
        adab = A.tile([2, 6 * D], F32, "adab"); modsb = A.tile([2, 6 * D], F32, "modsb")
        wa = [A.tile([128, 8, 512], F32, "wa") for _ in range(3)]
        dma("sync", cl[:], c_in.rearrange("(j p) -> p j", p=128), "cl", [], ["cl"], allow_slow_non_contiguous=True)
        dma("sync", cc[:], cctx_in.rearrange("(j p) -> p j", p=128), "cc", [], ["cc"], allow_slow_non_contiguous=True)
        act(sct[:, :, 0], cl[:], AF.Silu, ["cl"], ["sct0"])
        act(sct[:, :, 1], cc[:], AF.Silu, ["cc"], ["sct1"])
        it = 0
        for l in range(2):
            dma("sync", adab[:], ada_b[l:l + 1, :].to_broadcast([2, 6 * D]), "adab", [], ["adab"])
            awv = ada_w[l].rearrange("(j p) n -> p j n", p=128)
            for n in range(12):
                s = it % 3; b = it % 2; it += 1
                dma("sync" if it % 2 else "scalar", wa[s][:], awv[:, :, n * 512:(n + 1) * 512], "wa%d" % s, [], ["wa%d" % s])
                for j in range(8):
                    mm(psb[b][0:2, :], sct[:, j, :], wa[s][:, j, :], j == 0, j == 7, ["sct0", "sct1", "wa%d" % s], ["ps%d" % b])
                tt("vector", modsb[:, n * 512:(n + 1) * 512], psb[b][0:2, :], adab[:, n * 512:(n + 1) * 512], ALU.add,
                   ["ps%d" % b, "adab"], ["modsb"])
            dma("sync", modrow[l], modsb[:], "modsb", ["modsb"], ["modrow"])

    def norm_setup(l, which, sets):
        ng = norm1_g if which == 1 else norm2_g
        o_sh = 0 if which == 1 else 3
        res = {}
        gb = A.tile([128, D], F32, "gb")
        bcload(gb, ng[l:l + 1, :], D, "gb")
        for s in sets:
            gs = A.tile([128, D], F32, "gs"); sh = A.tile([128, D], F32, "sh")
            bcload(gs, modrow[l, s:s + 1, (o_sh + 1) * D:(o_sh + 2) * D], D, "gs%d" % s, eng="scalar")
            bcload(sh, modrow[l, s:s + 1, o_sh * D:(o_sh + 1) * D], D, "sh%d" % s)
            stt("vector", gs[:], gs[:], 1.0, gb[:], ALU.add, ALU.mult, ["gs%d" % s, "gb"], ["gs%d" % s])
            res[s] = (gs, sh, "gs%d" % s, "sh%d" % s)
        return res

    def norm_alloc():
        return dict(
            xt=[A.tile([128, D], F32, "xt") for _ in range(2)],
            y=[A.tile([128, D], F32, "y") for _ in range(2)],
            nb=[A.tile([128, D], BF16, "nb") for _ in range(2)],
            ss=[A.tile([128, 1], F32, "ss") for _ in range(2)],
            rstd=[A.tile([128, 1], F32, "rstd") for _ in range(2)],
        )

    def norm_tile(nbuf, i, t, gsh, src=None):
        s = i % 2
        xt, y, nb, ss, rstd = nbuf["xt"][s], nbuf["y"][s], nbuf["nb"][s], nbuf["ss"][s], nbuf["rstd"][s]
        gs, sh, kgs, ksh = gsh
        k = "n%d" % s
        srcap = h[t * 128:(t + 1) * 128, :] if src is None else src
        dma("sync" if i % 2 == 0 else "scalar", xt[:], srcap, k + "xt", [("h", t)], [k + "xt"])
        act(nb[:], xt[:], AF.Square, [k + "xt"], [k + "nb", k + "ss"], accum_out=ss[:])
        rstd_from_ss(ss[:], rstd[:], D, k)
        stt("vector", y[:], xt[:], rstd[:, 0:1], gs[:], ALU.mult, ALU.mult, [k + "xt", k + "rstd", kgs], [k + "y"])
        tt("vector", nb[:], y[:], sh[:], ALU.add, [k + "y", ksh], [k + "nb"])
        return s

    def transpose_to(nb_ap, rows, dst_ap, bank, r, w, eng="scalar"):
        pv = psbf(bank)
        for j in range(8):
            tr(pv[:, j, 0:rows], nb_ap[:, j * 128:(j + 1) * 128], ident_bf[0:rows, 0:rows], r + ["identb"], ["ps%d" % bank])
        cp(eng, dst_ap, pv[:, :, 0:rows], ["ps%d" % bank], w)

    def phase_attn(l=0):
        lam_init = 0.8 - 0.6 * math.exp(-0.3 * l)
        A.reset(base_mark)
        QT = A.tile([128, 8, 2304], BF16, "QT"); KT = A.tile([128, 8, 2304], BF16, "KT")
        Vaug = A.tile([128, NT, 8, 129], BF16, "Vaug")
        m1 = A.mark()
        nT = A.tile([128, 8, 2304], BF16, "nT")
        gsh = norm_setup(l, 1, (0, 1))
        nbuf = norm_alloc()
        for i, t in enumerate(range(NT)):
            s = norm_tile(nbuf, i, t, gsh[0 if t < NTL else 1])
            transpose_to(nbuf["nb"][s][:], 128, nT[:, :, t * 128:(t + 1) * 128], 4 + i % 2, ["n%dnb" % s], [("nT", t)],
                         eng="scalar" if i % 2 else "vector")
        P.barrier()
        if attn_stop == "norm":
            return
        A.reset(m1 + 8 * 2304 * 2)
        nT_all = [("nT", t) for t in range(NT)]
        wq = [A.tile([128, 8, D], BF16, "wq") for _ in range(2)]
        ropeC = A.tile([128, 2048], F32, "ropeC"); ropeS = A.tile([128, 2048], F32, "ropeS")
        t1 = [A.tile([128, 512], F32, "t1") for _ in range(2)]
        t2 = [A.tile([128, 512], F32, "t2") for _ in range(2)]
        dma("sync", ropeC[:], rope_c, "ropeC", [], ["ropeC"])
        dma("scalar", ropeS[:], rope_s, "ropeS", [], ["ropeS"])
        P.op("gpsimd", lambda e: e.memset(Vaug[:, :, :, 128:129], 1.0), [], ["Vones"])
        it = 0
        wv5 = [wq[i][:].rearrange("p j (b h f) -> p j b h f", h=2, f=16) for i in range(2)]
        for part, dstT, dk in ((0, QT, "QT"), (1, KT, "KT")):
            cast_rows(wq[0], w_qkv, part * D, D, "wq0")
            for hh in range(2):
                for j in range(8):
                    cp("scalar" if (j + hh) % 2 else "vector", wv5[1][:, j, :, hh, :], wv5[0][:, j, :, 1 - hh, :], [("wq0", j, 0)], [("wq1", j, hh)])
            for c in range(8):
                for n in range(5):
                    n0, nw = (n * 512, 512) if n < 4 else (2048, 256)
                    b = it % 2; it += 1
                    for j in range(8):
                        mm(psb[b][:, 0:nw], wq[0][:, j, c * 128:(c + 1) * 128], nT[:, j, n0:n0 + nw], j == 0, j == 7,
                           [("wq0", j, 0)] + nT_all, ["ps%d" % b])
                    if n == 4:
                        cp("scalar", dstT[:, c, n0:n0 + nw], psb[b][:, 0:nw], ["ps%d" % b], [(dk, c, n)])
                    else:
                        for j in range(8):
                            mm(psb[2 + b][:, :], wq[1][:, j, c * 128:(c + 1) * 128], nT[:, j, n0:n0 + nw], j == 0, j == 7,
                               [("wq1", j, 0), ("wq1", j, 1)] + nT_all, ["ps%d" % (2 + b)])
                        tt("vector", t1[b][:], psb[b][:, :], ropeC[:, n0:n0 + 512], ALU.mult, ["ps%d" % b, "ropeC"], ["t1%d" % b])
                        tt("vector", t2[b][:], psb[2 + b][:, :], ropeS[:, n0:n0 + 512], ALU.mult, ["ps%d" % (2 + b), "ropeS"], ["t2%d" % b])
                        tt("vector", dstT[:, c, n0:n0 + 512], t1[b][:], t2[b][:], ALU.add, ["t1%d" % b, "t2%d" % b], [(dk, c, n)])
        if attn_stop in ("proj_norope", "proj_qk"):
            P.barrier()
            return
        cast_rows(wq[0], w_qkv, 2 * D, D, "wq0")
        for t in range(NT):
            for n in range(2):
                b = it % 2; it += 1
                for j in range(8):
                    mm(psb[b][:, :], nT[:, j, t * 128:(t + 1) * 128], wq[0][:, j, n * 512:(n + 1) * 512], j == 0, j == 7,
                       [("wq0", j, 0)] + nT_all, ["ps%d" % b])
                cp("scalar" if it % 2 else "vector", Vaug[:, t, n * 4:(n + 1) * 4, 0:128],
                   psb[b][:].rearrange("p (a b) -> p a b", a=4), ["ps%d" % b], [("V", t, n)])
        P.barrier()
        if attn_stop == "proj":
            return
        A.reset(m1)
        ON = A.tile([128, NT, D], BF16, "ON")
        m2 = A.mark()
        ET = [A.tile([128, 512], BF16, "ET") for _ in range(3)]
        O1 = A.tile([128, 4, 128], F32, "O1")
        od = [A.tile([128, 128], F32, "od") for _ in range(2)]
        junk = [A.tile([128, 128], BF16, "junk") for _ in range(2)]
        rs = [A.tile([128, 1], F32, "rs") for _ in range(4)]
        rs2 = [A.tile([128, 1], F32, "rs2") for _ in range(2)]
        ss2 = [A.tile([128, 1], F32, "ss2") for _ in range(2)]
        r2 = [A.tile([128, 1], F32, "r2") for _ in range(2)]
        lqk = [A.tile([128, 64], F32, "lqk") for _ in range(4)]
        lpr = A.tile([128, 64], F32, "lpr")
        lsum = A.tile([128, 2], F32, "lsum")
        neglam = A.tile([128, 1], F32, "neglam")
        sg = A.tile([128, 128], F32, "sg")
        for i, src in enumerate((lq1, lk1, lq2, lk2)):
            bcload(lqk[i], src, 64, "lqk%d" % i)
        bcload(sg, subln_g, 128, "sg")
        ts("vector", sg[:], sg[:], 1.0 - lam_init, None, ALU.mult, None, ["sg"], ["sg"])
        for i in range(2):
            tt("vector", lpr[:], lqk[2 * i][:], lqk[2 * i + 1][:], ALU.mult, ["lqk%d" % (2 * i), "lqk%d" % (2 * i + 1)], ["lpr"])
            P.op("vector", lambda e, i=i: e.reduce_sum(out=lsum[:, i:i + 1], in_=lpr[:], axis=AX.X), ["lpr"], ["lsum%d" % i])
        act(lsum[:], lsum[:], AF.Exp, ["lsum0", "lsum1"], ["lsum0", "lsum1"])
        tt("vector", neglam[:], lsum[:, 1:2], lsum[:, 0:1], ALU.subtract, ["lsum0", "lsum1"], ["neglam"])
        ts("vector", neglam[:], neglam[:], -lam_init, None, ALU.add, None, ["neglam"], ["neglam"])
        si = 0; ei = 0; oi = 0
        qchunks = [(n * 512, 512, list(range(NT))) for n in range(4)] + [(2048, 256, [16, 17])]
        for hd in range(8):
            for (q0, qw, ktiles) in qchunks:
                nq = qw // 128
                for comp in range(2):
                    pb = comp * 64
                    def pv(ki, kt, e_):
                        for qs in range(nq):
                            mm(psb[4 + qs][:, 0:129], ET[e_][:, qs * 128:(qs + 1) * 128], Vaug[:, kt, hd, :],
                               ki == 0, ki == len(ktiles) - 1, ["ET%d" % e_], ["ps%d" % (4 + qs)])
                    prev = None
                    for ki, kt in enumerate(ktiles):
                        sbk = si % 2; si += 1
                        mm(psb[sbk][:, 0:qw], KT[pb:pb + 64, hd, kt * 128:(kt + 1) * 128], QT[pb:pb + 64, hd, q0:q0 + qw],
                           True, True, [], ["ps%d" % sbk])
                        e_ = ei % 3; ei += 1
                        act(ET[e_][:, 0:qw], psb[sbk][:, 0:qw], AF.Exp, ["ps%d" % sbk], ["ET%d" % e_], scale=0.125)
                        if prev is not None:
                            pv(*prev)
                        prev = (ki, kt, e_)
                    pv(*prev)
                    for qs in range(nq):
                        qt = q0 // 128 + qs
                        pk = "ps%d" % (4 + qs)
                        P.op("vector", lambda e, qs=qs: e.reciprocal(out=rs[qs][:], in_=psb[4 + qs][:, 128:129]), [pk], ["rs%d" % qs])
                        if comp == 0:
                            ts("vector", O1[:, qs, :], psb[4 + qs][:, 0:128], rs[qs][:, 0:1], None, ALU.mult, None,
                               [pk, "rs%d" % qs], ["O1%d" % qs])
                        else:
                            o = oi % 2; oi += 1
                            tt("vector", rs2[o][:], rs[qs][:], neglam[:], ALU.mult, ["rs%d" % qs, "neglam"], ["rs2%d" % o])
                            stt("vector", od[o][:], psb[4 + qs][:, 0:128], rs2[o][:, 0:1], O1[:, qs, :], ALU.mult, ALU.add,
                                [pk, "rs2%d" % o, "O1%d" % qs], ["od%d" % o])
                            act(junk[o][:], od[o][:], AF.Square, ["od%d" % o], ["junk%d" % o, "a%dss" % o], accum_out=ss2[o][:])
                            ts("vector", r2[o][:], ss2[o][:], 1.0 / 128, EPS, ALU.mult, ALU.add, ["a%dss" % o], ["r2%d" % o])
                            act(r2[o][:], r2[o][:], AF.Sqrt, ["r2%d" % o], ["r2%d" % o])
                            P.op("vector", lambda e, o=o: e.reciprocal(out=r2[o][:], in_=r2[o][:]), ["r2%d" % o], ["r2%d" % o])
                            ts("vector", od[o][:], od[o][:], r2[o][:, 0:1], None, ALU.mult, None, ["od%d" % o, "r2%d" % o], ["od%d" % o])
                            tt("vector", ON[:, qt, hd * 128:(hd + 1) * 128], od[o][:], sg[:], ALU.mult,
                               ["od%d" % o, "sg"], [("ON", qt, hd)])
        P.barrier()
        if attn_stop == "core":
            return
        A.reset(m2)
        wo = A.tile([128, 8, D], BF16, "wo")
        cast_rows(wo, w_o, 0, D, "wo")
        out_proj(lambda t, slot: ON[:, t, :], None, wo, l, list(range(NT)), transpose=True)

    def out_proj(src_fn, zT, wo, l, tiles, transpose):
        g1 = {}
        for s in ((0, 1) if len(tiles) > NTL else (0,)):
            g1[s] = A.tile([128, D], F32, "g1bc")
            bcload(g1[s], modrow[l, s:s + 1, 2 * D:3 * D], D, "g1bc%d" % s)
        ONT = [A.tile([128, 8, 128], BF16, "ONT") for _ in range(2)]
        ht = [A.tile([128, D], F32, "ht") for _ in range(2)]
        tmp = [A.tile([128, D], F32, "tmp") for _ in range(2)]
        it = 0
        for i, t in enumerate(tiles):
            s = i % 2
            st = 0 if t < NTL else 1
            dma("sync" if s == 0 else "scalar", ht[s][:], h[t * 128:(t + 1) * 128, :], "ht%d" % s, [], ["ht%d" % s])
            if transpose:
                transpose_to(src_fn(t, s), 128, ONT[s][:], 2 + s, [], ["ONT%d" % s])
            for n in range(2):
                b = it % 2; it += 1
                for j in range(8):
                    lhs = ONT[s][:, j, :] if transpose else zT[:, j, t * 128:(t + 1) * 128]
                    mm(psb[b][:, :], lhs, wo[:, j, n * 512:(n + 1) * 512], j == 0, j == 7,
                       [("wo", j, 0)] + (["ONT%d" % s] if transpose else []), ["ps%d" % b])
                tt("vector", tmp[s][:, n * 512:(n + 1) * 512], psb[b][:, :], g1[st][:, n * 512:(n + 1) * 512], ALU.mult,
                   ["ps%d" % b, "g1bc%d" % st], ["tmp%d%d" % (s, n)])
                tt("vector", ht[s][:, n * 512:(n + 1) * 512], ht[s][:, n * 512:(n + 1) * 512], tmp[s][:, n * 512:(n + 1) * 512], ALU.add,
                   ["tmp%d%d" % (s, n), "ht%d" % s], ["ht%d" % s])
            dma("sync" if s == 0 else "scalar", h[t * 128:(t + 1) * 128, :], ht[s][:], "ht%d" % s, ["ht%d" % s], [])

    def phase_moe(l, with_ctx):
        A.reset(base_mark)
        sets = (0, 1) if with_ctx else (0,)
        tiles = list(range(NT if with_ctx else NTL))
        ncap = [256, 32]
        probsT = A.tile([16, 2304], F32, "probsT")
        wr = A.tile([128, 8, NE], BF16, "wr")
        cast_rows(wr, w_router[l], 0, NE, "wr")
        idxT = A.tile([128, 3, NE], I32, "idxT"); gateT = A.tile([128, 3, NE], F32, "gateT")
        g2 = {}
        for s in sets:
            g2[s] = A.tile([128, D], F32, "g2bc")
            bcload(g2[s], modrow[l, s:s + 1, 5 * D:6 * D], D, "g2bc%d" % s)
        m0 = A.mark()
        gsh = norm_setup(l, 2, sets)
        nbuf = norm_alloc()
        nTt = [A.tile([128, 8, 128], BF16, "nTt") for _ in range(2)]
        lg = [A.tile([128, NE], F32, "lg") for _ in range(2)]
        mxl = [A.tile([128, 1], F32, "mxl") for _ in range(2)]
        sme = [A.tile([128, 1], F32, "sme") for _ in range(2)]
        for i, t in enumerate(tiles):
            s = norm_tile(nbuf, i, t, gsh[0 if t < NTL else 1])
            k = "r%d" % s
            dma("sync", mbf[t * 128:(t + 1) * 128, :], nbuf["nb"][s][:], "n%dnb" % s, ["n%dnb" % s], [("mbf", t)])
            transpose_to(nbuf["nb"][s][:], 128, nTt[s][:], 4 + s, ["n%dnb" % s], [k + "nTt"])
            for j in range(8):
                mm(psb[6 + s][:, 0:NE], nTt[s][:, j, :], wr[:, j, :], j == 0, j == 7, [k + "nTt", ("wr", j, 0)], ["ps%d" % (6 + s)])
            P.op("vector", lambda e, s=s: e.reduce_max(out=mxl[s][:], in_=psb[6 + s][:, 0:NE], axis=AX.X), ["ps%d" % (6 + s)], [k + "mx"])
            ts("vector", mxl[s][:], mxl[s][:], -1.0, None, ALU.mult, None, [k + "mx"], [k + "mx"])
            act(lg[s][:], psb[6 + s][:, 0:NE], AF.Exp, ["ps%d" % (6 + s), k + "mx"], [k + "lg", k + "sm"], bias=mxl[s][:, 0:1], accum_out=sme[s][:])
            P.op("vector", lambda e, s=s: e.reciprocal(out=sme[s][:], in_=sme[s][:]), [k + "sm"], [k + "sm"])
            ts("vector", lg[s][:], lg[s][:], sme[s][:, 0:1], None, ALU.mult, None, [k + "lg", k + "sm"], [k + "lg"])
            tr(psb[s][0:NE, 0:128], lg[s][:], ident_f[:], [k + "lg", "identf"], ["ps%d" % s])
            cp("vector", probsT[:, t * 128:(t + 1) * 128], psb[s][0:NE, 0:128], ["ps%d" % s], [("pT", t)])
        P.barrier()
        A.reset(m0)
        work = A.tile([16, 2048], F32, "work")
        gate = A.tile([16, 288], F32, "gate"); idxu = A.tile([16, 288], U32, "idxu"); idxf = A.tile([16, 288], F32, "idxf")
        for s in sets:
            n0, ntok, cap, c0 = (0, 2048, 256, 0) if s == 0 else (2048, 256, 32, 256)
            cp("vector", work[:, 0:ntok], probsT[:, n0:n0 + ntok], [], ["work"])
            for r in range(cap // 8):
                g_ = gate[:, c0 + r * 8:c0 + (r + 1) * 8]
                P.op("vector", lambda e, g_=g_, ntok=ntok: e.max(out=g_, in_=work[:, 0:ntok]), ["work"], ["gate"])
                P.op("vector", lambda e, g_=g_, ntok=ntok, r=r, c0=c0: e.max_index(out=idxu[:, c0 + r * 8:c0 + (r + 1) * 8], in_max=g_, in_values=work[:, 0:ntok]),
                     ["work", "gate"], ["idxu"])
                P.op("vector", lambda e, g_=g_, ntok=ntok: e.match_replace(out=work[:, 0:ntok], in_to_replace=g_, in_values=work[:, 0:ntok], imm_value=-1.0),
                     ["work", "gate"], ["work"])
        ncol = 288 if with_ctx else 256
        cp("vector", idxf[:, 0:ncol], idxu[:, 0:ncol], ["idxu"], ["idxf"])
        if with_ctx:
            ts("vector", idxf[:, 256:288], idxf[:, 256:288], 2048.0, None, ALU.add, None, ["idxf"], ["idxf"])
        parts = [(0, 0, 128), (1, 128, 128)] + ([(2, 256, 32)] if with_ctx else [])
        for (pi, c0, rows) in parts:
            tr(psb[0][0:rows, 0:NE], idxf[:, c0:c0 + rows], ident_f[0:NE, 0:NE], ["idxf", "identf"], ["ps0"])
            cp("vector", idxT[0:rows, pi, :], psb[0][0:rows, 0:NE], ["ps0"], ["idxT"])
            tr(psb[1][0:rows, 0:NE], gate[:, c0:c0 + rows], ident_f[0:NE, 0:NE], ["gate", "identf"], ["ps1"])
            cp("vector", gateT[0:rows, pi, :], psb[1][0:rows, 0:NE], ["ps1"], ["gateT"])
        P.barrier()
        A.reset(m0)
        ntk = 288 if with_ctx else 256
        Xg = [[A.tile([128, D], BF16, "Xg") for _ in parts] for _ in range(2)]
        XgT = [A.tile([128, 8, ntk], BF16, "XgT") for _ in range(2)]
        NWG = 4
        wg = [A.tile([128, 8, 1024], BF16, "wg") for _ in range(NWG)]
        wd = [A.tile([128, 2, D], BF16, "wd") for _ in range(NWG)]
        hT = A.tile([128, 22, ntk], BF16, "hT")
        sgt = [A.tile([128, ntk], F32, "sgt") for _ in range(2)]
        ysb = [A.tile([128, D], F32, "ysb") for _ in range(len(parts) * 2)]

        def gather(e):
            sl = e % 2
            for (pi, c0, rows) in parts:
                P.dma("gpsimd", lambda en, pi=pi, rows=rows, sl=sl, e=e: en.indirect_dma_start(
                    out=Xg[sl][pi][0:rows, :], out_offset=None, in_=mbf[:, :],
                    in_offset=bass.IndirectOffsetOnAxis(ap=idxT[0:rows, pi, e:e + 1], axis=0)),
                    "Xg%d%d" % (sl, pi), [], ["Xg%d%d" % (sl, pi)])

        def xpose(e):
            sl = e % 2
            for (pi, c0, rows) in parts:
                transpose_to(Xg[sl][pi][0:rows, :], rows, XgT[sl][:, :, c0:c0 + rows], 2 + pi % 2,
                             ["Xg%d%d" % (sl, pi)], [("XgT", sl, pi)], eng="vector")

        wgi = [0]; wdi = [0]; gi = [0]; yi = [0]

        def load_wg(e, b):
            s = wgi[0] % NWG; wgi[0] += 1
            nf = 512 if b < 5 else 256
            keys = []
            cast_rows(wg[s], w_gu[l, e], b * 512, nf, "wg%d" % s, 0, defer=keys)
            cast_rows(wg[s], w_gu[l, e], FE + b * 512, nf, "wg%d" % s, 512, defer=keys)
            cast_finalize("wg%d" % s, keys)
            return s

        def load_wd(e, b):
            s = wdi[0] % NWG; wdi[0] += 1
            cast_rows(wd[s], w_dn[l, e][2 * b * 128:(2 * b + 2) * 128, :], 0, D, "wd%d" % s)
            return s

        blocks = []
        for e in range(NE):
            blocks += [("g", e, b) for b in range(6)] + [("d", e, b) for b in range(11)]
        PF = 3
        slots = {}
        gather(0)
        xpose(0)
        for bi in range(min(PF, len(blocks))):
            kind, e, b = blocks[bi]
            slots[bi] = load_wg(e, b) if kind == "g" else load_wd(e, b)
        for bi, (kind, e, b) in enumerate(blocks):
            nb_ = bi + PF
            if nb_ < len(blocks):
                k2, e2, b2 = blocks[nb_]
                slots[nb_] = load_wg(e2, b2) if k2 == "g" else load_wd(e2, b2)
            sl = e % 2
            xk = [("XgT", sl, pi) for (pi, _, _) in parts]
            if kind == "g":
                if b == 0 and e + 1 < NE:
                    gather(e + 1)
                s = slots[bi]
                for sub in range(4 if b < 5 else 2):
                    c = b * 4 + sub
                    gb = gi[0] % 2; gi[0] += 1
                    for gu, colb in ((0, sub * 128), (1, 512 + sub * 128)):
                        bank = gb * 2 + gu
                        for j in range(8):
                            mm(psb[bank][:, 0:ntk], wg[s][:, j, colb:colb + 128], XgT[sl][:, j, :], j == 0, j == 7,
                               [("wg%d" % s, j, 0 if gu == 0 else 512)] + xk, ["ps%d" % bank])
                    act(sgt[gb][:], psb[gb * 2][:, 0:ntk], AF.Silu, ["ps%d" % (gb * 2)], ["sgt%d" % gb])
                    tt("vector", hT[:, c, :], sgt[gb][:], psb[gb * 2 + 1][:, 0:ntk], ALU.mult,
                       ["sgt%d" % gb, "ps%d" % (gb * 2 + 1)], [("hT", c)])
            else:
                s = slots[bi]
                if b == 0 and e + 1 < NE:
                    xpose(e + 1)
                for cc in range(2):
                    c = 2 * b + cc
                    for (pi, c0, rows) in parts:
                        for dh in range(2):
                            bank = (4 + pi * 2 + dh) if pi < 2 else dh
                            mm(psb[bank][0:rows, :], hT[:, c, c0:c0 + rows], wd[s][:, cc, dh * 512:(dh + 1) * 512],
                               c == 0, c == 21, [("wd%d" % s, cc, 0), ("hT", c)], ["ps%d" % bank])
                if b == 10:
                    for (pi, c0, rows) in parts:
                        yb = yi[0] % len(ysb); yi[0] += 1
                        st = 0 if pi < 2 else 1
                        for dh in range(2):
                            bank = (4 + pi * 2 + dh) if pi < 2 else dh
                            stt("vector", ysb[yb][0:rows, dh * 512:(dh + 1) * 512], psb[bank][0:rows, :], gateT[0:rows, pi, e:e + 1],
                                g2[st][0:rows, dh * 512:(dh + 1) * 512], ALU.mult, ALU.mult,
                                ["ps%d" % bank, "gateT", "g2bc%d" % st], ["ysb%d" % yb])
                        P.dma("gpsimd", lambda en, pi=pi, rows=rows, yb=yb, e=e: en.indirect_dma_start(
                            out=h[:, :], out_offset=bass.IndirectOffsetOnAxis(ap=idxT[0:rows, pi, e:e + 1], axis=0),
                            in_=ysb[yb][0:rows, :], in_offset=None, compute_op=ALU.add),
                            "ysb%d" % yb, ["ysb%d" % yb] + [("hsc", e - 1, p2) for (p2, _, _) in parts], [("hsc", e, pi)])

    def phase_lru(l=1):
        A.reset(base_mark)
        nT = A.tile([128, 8, 2304], BF16, "nT")
        zT = A.tile([128, 8, 2048], BF16, "zT")
        m1 = A.mark()
        gsh = norm_setup(l, 1, (0, 1))
        nbuf = norm_alloc()
        for i, t in enumerate(range(NT)):
            s = norm_tile(nbuf, i, t, gsh[0 if t < NTL else 1])
            transpose_to(nbuf["nb"][s][:], 128, nT[:, :, t * 128:(t + 1) * 128], 4 + i % 2, ["n%dnb" % s], [("nT", t)],
                         eng="scalar" if i % 2 else "vector")
        P.barrier()
        A.reset(m1)
        nT_all = []
        win = A.tile([128, 8, 2 * D], BF16, "win")
        cast_rows(win, lru_w_in, 0, 2 * D, "win")
        wgt = A.tile([128, 2, 8, 256], BF16, "wgt")
        for d_ in range(2):
            for k_ in range(8):
                dma("gpsimd", wgt[:, d_, k_, :], w_gates[d_, k_], "wgt", [], [("wgt", d_, k_)])
        cast_finalize("wgt", [("wgt", d_, k_) for d_ in range(2) for k_ in range(8)])
        bin_ = A.tile([128, 16], F32, "bin")
        dma("sync", bin_[:], lru_b_in.rearrange("(j p) -> p j", p=128), "bin", [], ["bin"], allow_slow_non_contiguous=True)
        cw = A.tile([128, 4, 8], F32, "cw")
        dma("sync", cw[:], conv_w.rearrange("j (k p) -> p j k", p=128), "cw", [], ["cw"], allow_slow_non_contiguous=True)
        cb = A.tile([128, 8], F32, "cb")
        dma("sync", cb[:], conv_b.rearrange("(k p) -> p k", p=128), "cb", [], ["cb"], allow_slow_non_contiguous=True)
        bg = A.tile([128, 2, 8, 2], F32, "bg")
        dma("sync", bg[:], b_gates.rearrange("d k (hf p) -> p d k hf", p=128), "bg", [], ["bg"], allow_slow_non_contiguous=True)
        lamt = A.tile([128, 2, 8], F32, "lamt")
        dma("sync", lamt[:], lru_lam.rearrange("d (k p) -> p d k", p=128), "lamt", [], ["lamt"], allow_slow_non_contiguous=True)
        sdec = A.tile([128, 2, 8], F32, "sdec"); sdec2 = A.tile([128, 2, 8], F32, "sdec2")
        act(sdec[:], lamt[:], AF.Exp, ["lamt"], ["sdec"], scale=-1.0)
        act(sdec[:], sdec[:], AF.Ln, ["sdec"], ["sdec"], bias=1.0)
        ts("vector", sdec2[:], sdec[:], -16.0, None, ALU.mult, None, ["sdec"], ["sdec2"])
        ts("vector", sdec[:], sdec[:], -8.0, None, ALU.mult, None, ["sdec", "sdec2"], ["sdec"])
        LC, LL = 256, 2048
        xpc = A.tile([128, LC + 3], F32, "xpc"); xpl = A.tile([128, LL + 3], F32, "xpl")
        xc = A.tile([128, 2304], F32, "xc")
        xb = A.tile([128, 2304], BF16, "xb")
        yg = A.tile([128, 2048], F32, "yg")
        rg = A.tile([128, 2304], F32, "rg"); ig = A.tile([128, 2304], F32, "ig")
        at = A.tile([128, 2304], F32, "at"); qt_ = A.tile([128, 2304], F32, "qt")
        hf = A.tile([128, 2304], F32, "hf"); hb = A.tile([128, 2304], F32, "hb")
        for tl in (xpc, xpl):
            P.op("gpsimd", lambda e, tl=tl: e.memset(tl[:], 0.0), [], ["xp"])
        it = 0
        chunks = [(n * 512, 512) for n in range(4)] + [(2048, 256)]
        for k in range(8):
            for (n0, nw) in chunks:
                b = it % 2; it += 1
                for j in range(8):
                    mm(psb[b][:, 0:nw], win[:, j, D + k * 128:D + (k + 1) * 128], nT[:, j, n0:n0 + nw], j == 0, j == 7, [("win", j, 1024)], ["ps%d" % b])
                dst = xpl[:, 2 + n0:2 + n0 + nw] if n0 < 2048 else xpc[:, 2:2 + LC]
                act(dst, psb[b][:, 0:nw], AF.Identity, ["ps%d" % b, "bin"], ["xp"], bias=bin_[:, 8 + k:9 + k])
                if n0 < 2048:
                    b = it % 2; it += 1
                    for j in range(8):
                        mm(psb[b][:, 0:nw], win[:, j, k * 128:(k + 1) * 128], nT[:, j, n0:n0 + nw], j == 0, j == 7, [("win", j, 0)], ["ps%d" % b])
                    act(yg[:, n0:n0 + nw], psb[b][:, 0:nw], AF.Gelu_apprx_tanh, ["ps%d" % b, "bin"], ["yg"], bias=bin_[:, k:k + 1])
            for (xp, o0, L) in ((xpc, 0, LC), (xpl, LC, LL)):
                ts("vector", xc[:, o0:o0 + L], xp[:, 0:L], cw[:, 0, k:k + 1], cb[:, k:k + 1], ALU.mult, ALU.add, ["xp", "cw", "cb"], ["xc"])
                for j in range(1, 4):
                    stt("vector", xc[:, o0:o0 + L], xp[:, j:j + L], cw[:, j, k:k + 1], xc[:, o0:o0 + L], ALU.mult, ALU.add, ["xp", "cw", "xc"], ["xc"])
            cp("vector", xb[:], xc[:], ["xc"], ["xb"])
            for d in range(2):
                for half, dstt, dk in ((0, rg, "rg"), (1, ig, "ig")):
                    for (n0, nw) in [(0, 256)] + [(256 + n * 512, 512) for n in range(4)]:
                        b = it % 2; it += 1
                        mm(psb[b][:, 0:nw], wgt[:, d, k, half * 128:(half + 1) * 128], xb[:, n0:n0 + nw], True, True, [("wgt", d, k), "xb"], ["ps%d" % b])
                        act(dstt[:, n0:n0 + nw], psb[b][:, 0:nw], AF.Sigmoid, ["ps%d" % b, "bg"], [dk], bias=bg[:, d, k, half:half + 1])
                act(at[:], rg[:], AF.Exp, ["rg", "sdec"], ["at"], scale=sdec[:, d, k:k + 1])
                act(qt_[:], rg[:], AF.Exp, ["rg", "sdec2"], ["qt"], scale=sdec2[:, d, k:k + 1])
                act(qt_[:], qt_[:], AF.Sqrt, ["qt"], ["qt"], scale=-1.0, bias=1.0)
                tt("vector", ig[:], ig[:], xc[:], ALU.mult, ["ig", "xc"], ["ig"])
                tt("vector", ig[:], ig[:], qt_[:], ALU.mult, ["ig", "qt"], ["ig"])
                hh, hk = (hf, "hf") if d == 0 else (hb, "hb")
                if d == 0:
                    P.op("vector", lambda e, hh=hh: e.tensor_tensor_scan(out=hh[:, 0:LC], data0=at[:, 0:LC], data1=ig[:, 0:LC], initial=0.0,
                                                                     op0=ALU.mult, op1=ALU.add), ["at", "ig"], [hk])
                    P.op("vector", lambda e, hh=hh: e.tensor_tensor_scan(out=hh[:, LC:], data0=at[:, LC:], data1=ig[:, LC:], initial=hh[:, LC - 1:LC],
                                                                     op0=ALU.mult, op1=ALU.add), ["at", "ig", hk], [hk])
                else:
                    P.op("vector", lambda e, hh=hh: e.tensor_tensor_scan(out=hh[:, 0:LC][:, ::-1], data0=at[:, 0:LC][:, ::-1], data1=ig[:, 0:LC][:, ::-1],
                                                                     initial=0.0, op0=ALU.mult, op1=ALU.add), ["at", "ig"], [hk])
                    P.op("vector", lambda e, hh=hh: e.tensor_tensor_scan(out=hh[:, LC:][:, ::-1], data0=at[:, LC:][:, ::-1], data1=ig[:, LC:][:, ::-1],
                                                                     initial=hh[:, 0:1], op0=ALU.mult, op1=ALU.add), ["at", "ig", hk], [hk])
            tt("vector", hf[:, LC:], hf[:, LC:], hb[:, LC:], ALU.add, ["hf", "hb"], ["hf"])
            tt("vector", zT[:, k, :], hf[:, LC:], yg[:], ALU.mult, ["hf", "yg"], [("zT", k)])
        P.barrier()
        A.reset(m1)
        wo = A.tile([128, 8, D], BF16, "wo")
        cast_rows(wo, lru_w_out, 0, D, "wo")
        out_proj(None, zT, wo, l, list(range(NTL)), transpose=False)

    def phase_final():
        A.reset(base_mark)
        fg = A.tile([128, D], F32, "fg")
        bcload(fg, final_g, D, "fg")
        xt = [A.tile([128, D], F32, "fxt") for _ in range(2)]
        yo = [A.tile([128, D], F32, "fyo") for _ in range(2)]
        jk = [A.tile([128, D], BF16, "fjk") for _ in range(2)]
        ss = [A.tile([128, 1], F32, "fss") for _ in range(2)]
        rstd = [A.tile([128, 1], F32, "frs") for _ in range(2)]
        for t in range(NTL):
            s = t % 2; k = "f%d" % s
            dma("sync" if s == 0 else "scalar", xt[s][:], h[t * 128:(t + 1) * 128, :], k + "xt", [], [k + "xt"])
            act(jk[s][:], xt[s][:], AF.Square, [k + "xt"], [k + "jk", k + "ss"], accum_out=ss[s][:])
            rstd_from_ss(ss[s][:], rstd[s][:], D, k)
            stt("vector", yo[s][:], xt[s][:], rstd[s][:, 0:1], fg[:], ALU.mult, ALU.mult, [k + "xt", k + "rstd", "fg"], [k + "yo"])
            dma("sync" if s == 0 else "scalar", out[t * 128:(t + 1) * 128, :], yo[s][:], k + "yo", [k + "yo"], [])

    dma("sync", h[0:2048, :], x_in, "hinit0", [], [])
    dma("scalar", h[2048:2304, :], ctx_in, "hinit1", [], [])
    stages = ["mod", "attn", "moe0", "lru", "moe1", "final"]
    nst = len(stages) if stop_after is None else stages.index(stop_after) + 1
    for st in stages[:nst]:
        if st == "mod":
            phase_mod()
        elif st == "attn":
            phase_attn(0)
        elif st == "moe0":
            phase_moe(0, True)
        elif st == "lru":
            phase_lru(1)
        elif st == "moe1":
            phase_moe(1, False)
        elif st == "final":
            phase_final()
        P.barrier()
    P.wait_all("sync")
    P.emit()
    return nc


_CONST = {}


def make_in_maps(inputs):
    if "C" not in _CONST:
        C, S, perm = rope_tables()
        _CONST.update(C=C, S=S, perm=perm, ident=np.eye(128, dtype=np.float32))
    g = lambda k: np.ascontiguousarray(inputs[k])
    shared = {
        "c_ctx": g("c_ctx"), "ada_w": g("ada_w"), "ada_b": g("ada_b"), "norm1_g": g("norm1_g"), "norm2_g": g("norm2_g"),
        "final_g": g("final_g").reshape(1, D), "attn_w_qkv": g("attn_w_qkv")[0],
        "attn_lq1": g("attn_lq1"), "attn_lk1": g("attn_lk1"), "attn_lq2": g("attn_lq2"), "attn_lk2": g("attn_lk2"),
        "attn_subln_g": g("attn_subln_g"), "attn_w_o": g("attn_w_o")[0],
        "lru_w_in": g("lru_w_in")[0], "lru_b_in": g("lru_b_in")[0], "lru_conv_w": g("lru_conv_w")[0], "lru_conv_b": g("lru_conv_b")[0],
        "lru_w_gates": g("lru_w_gates")[0], "lru_b_gates": g("lru_b_gates")[0], "lru_lambda": g("lru_lambda")[0],
        "lru_w_out": g("lru_w_out")[0], "moe_w_router": g("moe_w_router"), "moe_w_gate_up": g("moe_w_gate_up"),
        "moe_w_down": g("moe_w_down"),
        "rope_c": _CONST["C"], "rope_s": _CONST["S"], "perm": _CONST["perm"], "ident": _CONST["ident"],
    }
    maps = []
    for b in range(8):
        m = dict(shared)
        m["x"] = g("x")[b]; m["ctx"] = g("ctx")[b]; m["c"] = g("c")[b]
        maps.append(m)
    return maps


def kernel(**inputs):
    nc = build_program()
    in_maps = make_in_maps(inputs)
    res = run_bass_kernel_spmd(nc, in_maps, core_ids=list(range(8)))
    return np.stack([np.asarray(r["out"]) for r in res.results], axis=0).astype(np.float32)
```

```python
import math
import numpy as np
import ml_dtypes
import concourse.bass as bass
import concourse.mybir as mybir
from concourse.bass_utils import run_bass_kernel_spmd

F32 = mybir.dt.float32
BF16 = mybir.dt.bfloat16
I32 = mybir.dt.int32
U32 = mybir.dt.uint32
AF = mybir.ActivationFunctionType
ALU = mybir.AluOpType
AX = mybir.AxisListType

ENGS = ["sync", "scalar", "vector", "gpsimd", "tensor"]
D = 1024
NTL, NTC, NT = 16, 2, 18
EPS = 1e-6
NE = 16
FE = 2816
DT_SIZE = {F32: 4, BF16: 2, I32: 4, U32: 4}


class Prog:
    def __init__(self, nc, same_engine_sync=("vector", "scalar", "gpsimd")):
        self.nc = nc
        self.stream = {e: [] for e in ENGS}
        self.ecount = {e: 0 for e in ENGS}
        self.chan = {}
        self.last_w = {}
        self.readers = {}
        self.waited = {e: {} for e in ENGS}
        self.same_engine_sync = set(same_engine_sync)
        self.sems = {}

    def _deps(self, reads, writes):
        toks = []
        for r in reads:
            t = self.last_w.get(r)
            if t is not None:
                toks.append(t)
        for w in writes:
            t = self.last_w.get(w)
            if t is not None:
                toks.append(t)
            toks.extend(self.readers.get(w, []))
        return toks

    def _record(self, tok, reads, writes):
        for r in reads:
            self.readers.setdefault(r, []).append(tok)
        for w in writes:
            self.last_w[w] = tok
            self.readers[w] = []

    def _emit_waits(self, eng, toks, all_same=False):
        need = {}
        for (k, v) in toks:
            if k == ("e", eng) and not all_same and eng not in self.same_engine_sync:
                continue
            if v > need.get(k, 0):
                need[k] = v
        for k, v in need.items():
            if self.waited[eng].get(k, 0) >= v:
                continue
            self.waited[eng][k] = v
            self.stream[eng].append(("wait", k, v))

    def op(self, eng, fn, reads=(), writes=()):
        reads = list(reads); writes = list(writes)
        self._emit_waits(eng, self._deps(reads, writes))
        self.ecount[eng] += 1
        tok = (("e", eng), self.ecount[eng])
        self.stream[eng].append(("ins", fn, ("e", eng), 1))
        self._record(tok, reads, writes)
        return tok

    def dma(self, eng, fn, chan, reads=(), writes=()):
        reads = list(reads); writes = list(writes)
        self._emit_waits(eng, self._deps(reads, writes))
        c = self.chan.setdefault(chan, [("c", chan), 0])
        c[1] += 16
        tok = (c[0], c[1])
        self.stream[eng].append(("ins", fn, c[0], 16))
        self._record(tok, reads, writes)
        return tok

    def _all_toks(self):
        toks = [(("e", e), self.ecount[e]) for e in ENGS if self.ecount[e] > 0]
        toks += [(c[0], c[1]) for c in self.chan.values()]
        return toks

    def barrier(self):
        toks = self._all_toks()
        for e in ENGS:
            self._emit_waits(e, toks, all_same=True)
        self.last_w = {}
        self.readers = {}

    def wait_all(self, eng):
        self._emit_waits(eng, self._all_toks(), all_same=True)

    def emit(self):
        nc = self.nc
        keys = [("e", e) for e in ENGS if self.ecount[e] > 0] + [c[0] for c in self.chan.values()]
        assert len(keys) <= 100, ("too many semaphores", len(keys))
        for i, k in enumerate(keys):
            self.sems[k] = nc.alloc_semaphore(name="sm%d" % i)
        sems = self.sems
        stream = self.stream

        def run(eng_name):
            def f(eng):
                for item in stream[eng_name]:
                    if item[0] == "wait":
                        eng.wait_ge(sems[item[1]], item[2])
                    else:
                        _, fn, k, inc = item
                        fn(eng).then_inc(sems[k], inc)
            return f

        with nc.Block() as block:
            block.sync(run("sync"))
            block.scalar(run("scalar"))
            block.vector(run("vector"))
            block.gpsimd(run("gpsimd"))
            block.tensor(run("tensor"))


class Arena:
    def __init__(self, nc, base=16512, top=229344):
        self.nc = nc; self.base = base; self.top = top; self.off = base; self.cnt = 0

    def mark(self):
        return self.off

    def reset(self, to=None):
        self.off = self.base if to is None else to

    def tile(self, shape, dt, name="t"):
        n = 1
        for s in shape[1:]:
            n *= s
        nbytes = (n * DT_SIZE[dt] + 31) // 32 * 32
        assert self.off + nbytes <= self.top, ("SBUF overflow", name, self.off, nbytes)
        self.cnt += 1
        t = self.nc.alloc_sbuf_tensor_at("%s_%d" % (name, self.cnt), list(shape), dt, offset=self.off)
        self.off += nbytes
        return t


def rope_tables():
    n = 2048
    row = np.repeat(np.arange(n // 64), 64).astype(np.float32)
    col = np.tile(np.arange(64), n // 64).astype(np.float32)
    inv = (np.float32(10000.0) ** (-(np.arange(16, dtype=np.float32) * np.float32(2.0)) / np.float32(32))).astype(np.float32)
    ang = np.concatenate([row[:, None] * inv, col[:, None] * inv], axis=-1).astype(np.float32)
    cos = np.cos(ang).astype(np.float32); sin = np.sin(ang).astype(np.float32)
    C = np.zeros((128, n), np.float32); S = np.zeros((128, n), np.float32)
    perm = np.zeros((128, 128), np.float32)
    for p in range(128):
        d = p % 64
        a = d // 32; half = (d % 32) // 16; f = d % 16
        C[p] = cos[:, a * 16 + f]
        S[p] = sin[:, a * 16 + f] * (-1.0 if half == 0 else 1.0)
        partner = p + 16 if half == 0 else p - 16
        perm[partner, p] = 1.0
    return C, S, perm


def build_program(debug=False, stop_after=None, attn_stop=None):
    nc = bass.Bass("TRN2", target_bir_lowering=False)
    P = Prog(nc)
    A = Arena(nc)

    def din(name, shape, dt=F32):
        return nc.dram_tensor(name, list(shape), dt, kind="ExternalInput").ap()

    x_in = din("x", [2048, D]); ctx_in = din("ctx", [256, D]); c_in = din("c", [D]); cctx_in = din("c_ctx", [D])
    ada_w = din("ada_w", [2, D, 6 * D]); ada_b = din("ada_b", [2, 6 * D])
    norm1_g = din("norm1_g", [2, D]); norm2_g = din("norm2_g", [2, D]); final_g = din("final_g", [1, D])
    w_qkv = din("attn_w_qkv", [D, 3 * D])
    lq1 = din("attn_lq1", [1, 64]); lk1 = din("attn_lk1", [1, 64]); lq2 = din("attn_lq2", [1, 64]); lk2 = din("attn_lk2", [1, 64])
    subln_g = din("attn_subln_g", [1, 128]); w_o = din("attn_w_o", [D, D])
    lru_w_in = din("lru_w_in", [D, 2 * D]); lru_b_in = din("lru_b_in", [2 * D])
    conv_w = din("lru_conv_w", [4, D]); conv_b = din("lru_conv_b", [D])
    w_gates = din("lru_w_gates", [2, 8, 128, 256]); b_gates = din("lru_b_gates", [2, 8, 256])
    lru_lam = din("lru_lambda", [2, D]); lru_w_out = din("lru_w_out", [D, D])
    w_router = din("moe_w_router", [2, D, NE]); w_gu = din("moe_w_gate_up", [2, NE, D, 2 * FE]); w_dn = din("moe_w_down", [2, NE, FE, D])
    rope_c = din("rope_c", [128, 2048]); rope_s = din("rope_s", [128, 2048]); perm_in = din("perm", [128, 128])
    ident_in = din("ident", [128, 128])
    out = nc.dram_tensor("out", [2048, D], F32, kind="ExternalOutput").ap()
    h = nc.dram_tensor("h_scr", [2304, D], F32, kind="ExternalOutput" if debug else "Internal").ap()
    modrow = nc.dram_tensor("modrow", [2, 2, 6 * D], F32, kind="ExternalOutput" if debug else "Internal").ap()
    mbf = nc.dram_tensor("mbf", [2304, D], BF16).ap()
    NPRE = [8, 7]
    wgu_bf = nc.dram_tensor("wgu_bf", [2, 8, D, 2 * FE], BF16).ap()
    wdn_bf = nc.dram_tensor("wdn_bf", [2, 8, FE, D], BF16).ap()

    pp = [nc.alloc_psum_tensor("pp%d" % i, [128, 1024], F32) for i in range(4)]
    psb = [pp[i // 2][:, (i % 2) * 512:(i % 2 + 1) * 512] for i in range(8)]

    def psbf(i):
        return psb[i][:].bitcast(BF16).rearrange("p (j n) -> p j n", j=8)

    def mm(o, lhsT, rhs, start, stop, r, w):
        P.op("tensor", lambda e: e.matmul(o, lhsT=lhsT, rhs=rhs, start=start, stop=stop), r, w)

    def tr(o, i, idn, r, w):
        P.op("tensor", lambda e: e.transpose(o, i, idn), r, w)

    def act(o, i, func, r, w, **kw):
        P.op("scalar", lambda e: e.activation(out=o, in_=i, func=func, **kw), r, w)

    def tt(eng, o, a, b, op, r, w):
        P.op(eng, lambda e: e.tensor_tensor(out=o, in0=a, in1=b, op=op), r, w)

    def ts(eng, o, a, s1, s2, op0, op1, r, w):
        if s2 is None:
            P.op(eng, lambda e: e.tensor_scalar(out=o, in0=a, scalar1=s1, scalar2=None, op0=op0), r, w)
        else:
            P.op(eng, lambda e: e.tensor_scalar(out=o, in0=a, scalar1=s1, scalar2=s2, op0=op0, op1=op1), r, w)

    def stt(eng, o, a, s, b, op0, op1, r, w):
        P.op(eng, lambda e: e.scalar_tensor_tensor(out=o, in0=a, scalar=s, in1=b, op0=op0, op1=op1), r, w)

    def cp(eng, o, i, r, w):
        if eng == "scalar":
            P.op(eng, lambda e: e.copy(out=o, in_=i), r, w)
        else:
            P.op(eng, lambda e: e.tensor_copy(out=o, in_=i), r, w)

    def dma(eng, o, i, chan, r, w, **kw):
        return P.dma(eng, lambda e: e.dma_start(out=o, in_=i, **kw), chan, r, w)

    def cast_finalize(chan, keys):
        c = P.chan[chan]
        for k_ in keys:
            P.last_w[k_] = (c[0], c[1])

    def cast_rows(dst3, src2, col0, ncols, chan, dcol0=0, defer=None):
        J = src2.shape[0] // 128
        keys = [] if defer is None else defer
        for j in range(J):
            for c in range(0, ncols, 1024):
                w_ = min(1024, ncols - c)
                dma("gpsimd", dst3[:, j, dcol0 + c:dcol0 + c + w_], src2[j * 128:(j + 1) * 128, col0 + c:col0 + c + w_], chan, [], [(chan, j, dcol0 + c)])
                keys.append((chan, j, dcol0 + c))
        if defer is None:
            cast_finalize(chan, keys)

    def precast(l, experts):
        ch = "pre%d" % l
        for e in experts:
            for j in range(8):
                for c0 in range(0, 2 * FE, 1024):
                    w_ = min(1024, 2 * FE - c0)
                    dma("gpsimd", wgu_bf[l, e, j * 128:(j + 1) * 128, c0:c0 + w_], w_gu[l, e, j * 128:(j + 1) * 128, c0:c0 + w_], ch, [], [])
            for c in range(22):
                dma("gpsimd", wdn_bf[l, e, c * 128:(c + 1) * 128, :], w_dn[l, e, c * 128:(c + 1) * 128, :], ch, [], [])

    def bcload(tile_, row_ap, n, chan, eng="sync"):
        dma(eng, tile_[:], row_ap.to_broadcast([128, n]), chan, [], [chan])

    ident_bf = A.tile([128, 128], BF16, "identb")
    ident_f = A.tile([128, 128], F32, "identf")
    dma("gpsimd", ident_bf[:], ident_in, "identb", [], ["identb"])
    dma("sync", ident_f[:], ident_in, "identf", [], ["identf"])
    base_mark = A.mark()

    def rstd_from_ss(ss, rstd, n, key):
        ts("vector", rstd, ss, 1.0 / n, EPS, ALU.mult, ALU.add, [key + "ss"], [key + "rstd"])
        act(rstd, rstd, AF.Sqrt, [key + "rstd"], [key + "rstd"])
        P.op("vector", lambda e: e.reciprocal(out=rstd, in_=rstd), [key + "rstd"], [key + "rstd"])

    def phase_mod():
        A.reset(base_mark)
        cl = A.tile([128, 8], F32, "cl"); cc = A.tile([128, 8], F32, "cc")
        sct = A.tile([128, 8, 2], F32, "sct")
        adab = A.tile([2, 6 * D], F32, "adab"); modsb = A.tile([2, 6 * D], F32, "modsb")
        wa = [A.tile([128, 8, 512], F32, "wa") for _ in range(3)]
        dma("sync", cl[:], c_in.rearrange("(j p) -> p j", p=128), "cl", [], ["cl"], allow_slow_non_contiguous=True)
        dma("sync", cc[:], cctx_in.rearrange("(j p) -> p j", p=128), "cc", [], ["cc"], allow_slow_non_contiguous=True)
        act(sct[:, :, 0], cl[:], AF.Silu, ["cl"], ["sct0"])
        act(sct[:, :, 1], cc[:], AF.Silu, ["cc"], ["sct1"])
        it = 0
        for l in range(2):
            dma("sync", adab[:], ada_b[l:l + 1, :].to_broadcast([2, 6 * D]), "adab", [], ["adab"])
            awv = ada_w[l].rearrange("(j p) n -> p j n", p=128)
            for n in range(12):
                s = it % 3; b = it % 2; it += 1
                dma("sync" if it % 2 else "scalar", wa[s][:], awv[:, :, n * 512:(n + 1) * 512], "wa%d" % s, [], ["wa%d" % s])
                for j in range(8):
                    mm(psb[b][0:2, :], sct[:, j, :], wa[s][:, j, :], j == 0, j == 7, ["sct0", "sct1", "wa%d" % s], ["ps%d" % b])
                tt("vector", modsb[:, n * 512:(n + 1) * 512], psb[b][0:2, :], adab[:, n * 512:(n + 1) * 512], ALU.add,
                   ["ps%d" % b, "adab"], ["modsb"])
            dma("sync", modrow[l], modsb[:], "modsb", ["modsb"], ["modrow"])

    def norm_setup(l, which, sets):
        ng = norm1_g if which == 1 else norm2_g
        o_sh = 0 if which == 1 else 3
        res = {}
        gb = A.tile([128, D], F32, "gb")
        bcload(gb, ng[l:l + 1, :], D, "gb")
        for s in sets:
            gs = A.tile([128, D], F32, "gs"); sh = A.tile([128, D], F32, "sh")
            bcload(gs, modrow[l, s:s + 1, (o_sh + 1) * D:(o_sh + 2) * D], D, "gs%d" % s, eng="scalar")
            bcload(sh, modrow[l, s:s + 1, o_sh * D:(o_sh + 1) * D], D, "sh%d" % s)
            stt("vector", gs[:], gs[:], 1.0, gb[:], ALU.add, ALU.mult, ["gs%d" % s, "gb"], ["gs%d" % s])
            res[s] = (gs, sh, "gs%d" % s, "sh%d" % s)
        return res

    def norm_alloc():
        return dict(
            xt=[A.tile([128, D], F32, "xt") for _ in range(2)],
            y=[A.tile([128, D], F32, "y") for _ in range(2)],
            nb=[A.tile([128, D], BF16, "nb") for _ in range(2)],
            ss=[A.tile([128, 1], F32, "ss") for _ in range(2)],
            rstd=[A.tile([128, 1], F32, "rstd") for _ in range(2)],
        )

    def norm_tile(nbuf, i, t, gsh, src=None):
        s = i % 2
        xt, y, nb, ss, rstd = nbuf["xt"][s], nbuf["y"][s], nbuf["nb"][s], nbuf["ss"][s], nbuf["rstd"][s]
        gs, sh, kgs, ksh = gsh
        k = "n%d" % s
        srcap = h[t * 128:(t + 1) * 128, :] if src is None else src
        dma("sync" if i % 2 == 0 else "scalar", xt[:], srcap, k + "xt", [("h", t)], [k + "xt"])
        act(nb[:], xt[:], AF.Square, [k + "xt"], [k + "nb", k + "ss"], accum_out=ss[:])
        rstd_from_ss(ss[:], rstd[:], D, k)
        stt("vector", y[:], xt[:], rstd[:, 0:1], gs[:], ALU.mult, ALU.mult, [k + "xt", k + "rstd", kgs], [k + "y"])
        tt("vector", nb[:], y[:], sh[:], ALU.add, [k + "y", ksh], [k + "nb"])
        return s

    def transpose_to(nb_ap, rows, dst_ap, bank, r, w, eng="scalar"):
        pv = psbf(bank)
        for j in range(8):
            tr(pv[:, j, 0:rows], nb_ap[:, j * 128:(j + 1) * 128], ident_bf[0:rows, 0:rows], r + ["identb"], ["ps%d" % bank])
        cp(eng, dst_ap, pv[:, :, 0:rows], ["ps%d" % bank], w)

    def phase_attn(l=0):
        lam_init = 0.8 - 0.6 * math.exp(-0.3 * l)
        A.reset(base_mark)
        QT = A.tile([128, 8, 2304], BF16, "QT"); KT = A.tile([128, 8, 2304], BF16, "KT")
        Vaug = A.tile([128, NT, 8, 129], BF16, "Vaug")
        m1 = A.mark()
        nT = A.tile([128, 8, 2304], BF16, "nT")
        gsh = norm_setup(l, 1, (0, 1))
        nbuf = norm_alloc()
        for i, t in enumerate(range(NT)):
            s = norm_tile(nbuf, i, t, gsh[0 if t < NTL else 1])
            transpose_to(nbuf["nb"][s][:], 128, nT[:, :, t * 128:(t + 1) * 128], 4 + i % 2, ["n%dnb" % s], [("nT", t)],
                         eng="scalar" if i % 2 else "vector")
        P.barrier()
        if attn_stop == "norm":
            return
        A.reset(m1 + 8 * 2304 * 2)
        nT_all = [("nT", t) for t in range(NT)]
        wq = [A.tile([128, 8, D], BF16, "wq") for _ in range(2)]
        ropeC = A.tile([128, 2048], F32, "ropeC"); ropeS = A.tile([128, 2048], F32, "ropeS")
        t1 = [A.tile([128, 512], F32, "t1") for _ in range(2)]
        t2 = [A.tile([128, 512], F32, "t2") for _ in range(2)]
        dma("sync", ropeC[:], rope_c, "ropeC", [], ["ropeC"])
        dma("scalar", ropeS[:], rope_s, "ropeS", [], ["ropeS"])
        P.op("gpsimd", lambda e: e.memset(Vaug[:, :, :, 128:129], 1.0), [], ["Vones"])
        it = 0
        pre_qkv_done = [False]
        wv5 = [wq[i][:].rearrange("p j (b h f) -> p j b h f", h=2, f=16) for i in range(2)]
        for part, dstT, dk in ((0, QT, "QT"), (1, KT, "KT")):
            cast_rows(wq[0], w_qkv, part * D, D, "wq0")
            if not pre_qkv_done[0]:
                pre_qkv_done[0] = True
                precast(0, [7])
            for hh in range(2):
                for j in range(8):
                    cp("scalar" if (j + hh) % 2 else "vector", wv5[1][:, j, :, hh, :], wv5[0][:, j, :, 1 - hh, :], [("wq0", j, 0)], [("wq1", j, hh)])
            for c in range(8):
                for n in range(5):
                    n0, nw = (n * 512, 512) if n < 4 else (2048, 256)
                    b = it % 2; it += 1
                    for j in range(8):
                        mm(psb[b][:, 0:nw], wq[0][:, j, c * 128:(c + 1) * 128], nT[:, j, n0:n0 + nw], j == 0, j == 7,
                           [("wq0", j, 0)] + nT_all, ["ps%d" % b])
                    if n == 4:
                        cp("scalar", dstT[:, c, n0:n0 + nw], psb[b][:, 0:nw], ["ps%d" % b], [(dk, c, n)])
                    else:
                        for j in range(8):
                            mm(psb[2 + b][:, :], wq[1][:, j, c * 128:(c + 1) * 128], nT[:, j, n0:n0 + nw], j == 0, j == 7,
                               [("wq1", j, 0), ("wq1", j, 1)] + nT_all, ["ps%d" % (2 + b)])
                        tt("vector", t1[b][:], psb[b][:, :], ropeC[:, n0:n0 + 512], ALU.mult, ["ps%d" % b, "ropeC"], ["t1%d" % b])
                        tt("vector", t2[b][:], psb[2 + b][:, :], ropeS[:, n0:n0 + 512], ALU.mult, ["ps%d" % (2 + b), "ropeS"], ["t2%d" % b])
                        tt("vector", dstT[:, c, n0:n0 + 512], t1[b][:], t2[b][:], ALU.add, ["t1%d" % b, "t2%d" % b], [(dk, c, n)])
        if attn_stop in ("proj_norope", "proj_qk"):
            P.barrier()
            return
        cast_rows(wq[0], w_qkv, 2 * D, D, "wq0")
        for t in range(NT):
            for n in range(2):
                b = it % 2; it += 1
                for j in range(8):
                    mm(psb[b][:, :], nT[:, j, t * 128:(t + 1) * 128], wq[0][:, j, n * 512:(n + 1) * 512], j == 0, j == 7,
                       [("wq0", j, 0)] + nT_all, ["ps%d" % b])
                cp("scalar" if it % 2 else "vector", Vaug[:, t, n * 4:(n + 1) * 4, 0:128],
                   psb[b][:].rearrange("p (a b) -> p a b", a=4), ["ps%d" % b], [("V", t, n)])
        P.barrier()
        if attn_stop == "proj":
            return
        precast(0, range(7))
        A.reset(m1)
        ON = A.tile([128, NT, D], BF16, "ON")
        m2 = A.mark()
        ET = [A.tile([128, 1024], BF16, "ET") for _ in range(3)]
        O1 = A.tile([128, 4, 128], F32, "O1")
        accs = [A.tile([128, 4, 129], F32, "accs") for _ in range(2)]
        od = [A.tile([128, 4, 128], F32, "od") for _ in range(2)]
        junk = [A.tile([128, 4, 128], F32, "junk") for _ in range(2)]
        rs = [A.tile([128, 4], F32, "rs") for _ in range(2)]
        rs2 = [A.tile([128, 4], F32, "rs2") for _ in range(2)]
        ss2 = [A.tile([128, 4], F32, "ss2") for _ in range(2)]
        r2 = [A.tile([128, 4], F32, "r2") for _ in range(2)]
        lqk = [A.tile([128, 64], F32, "lqk") for _ in range(4)]
        lpr = A.tile([128, 64], F32, "lpr")
        lsum = A.tile([128, 2], F32, "lsum")
        neglam = A.tile([128, 1], F32, "neglam")
        sg = A.tile([128, 128], F32, "sg")
        for i, src in enumerate((lq1, lk1, lq2, lk2)):
            bcload(lqk[i], src, 64, "lqk%d" % i)
        bcload(sg, subln_g, 128, "sg")
        ts("vector", sg[:], sg[:], 1.0 - lam_init, None, ALU.mult, None, ["sg"], ["sg"])
        for i in range(2):
            tt("vector", lpr[:], lqk[2 * i][:], lqk[2 * i + 1][:], ALU.mult, ["lqk%d" % (2 * i), "lqk%d" % (2 * i + 1)], ["lpr"])
            P.op("vector", lambda e, i=i: e.reduce_sum(out=lsum[:, i:i + 1], in_=lpr[:], axis=AX.X), ["lpr"], ["lsum%d" % i])
        act(lsum[:], lsum[:], AF.Exp, ["lsum0", "lsum1"], ["lsum0", "lsum1"])
        tt("vector", neglam[:], lsum[:, 1:2], lsum[:, 0:1], ALU.subtract, ["lsum0", "lsum1"], ["neglam"])
        ts("vector", neglam[:], neglam[:], -lam_init, None, ALU.add, None, ["neglam"], ["neglam"])
        si = 0; ei = 0; ai = 0
        qchunks = [(n * 512, 512, list(range(NT))) for n in range(4)] + [(2048, 256, [16, 17])]
        Qm = [[A.tile([128, 2304], BF16, "Qm") for _ in range(2)] for _ in range(2)]
        for sl_ in range(2):
            for c_ in range(2):
                P.op("vector", lambda e, sl_=sl_, c_=c_: e.memset(Qm[sl_][c_][:], 0.0), [], [("Qm", sl_, c_)])
        for hd in range(8):
            qsl = hd % 2
            for c_ in range(2):
                cp("vector", Qm[qsl][c_][c_ * 64:(c_ + 1) * 64, :], QT[c_ * 64:(c_ + 1) * 64, hd, :], [], [("Qm", qsl, c_)])
            for (q0, qw, ktiles) in qchunks:
                nq = qw // 128
                qt0 = q0 // 128
                pairs = [ktiles[i:i + 2] for i in range(0, len(ktiles), 2)]
                for comp in range(2):
                    pb = comp * 64

                    def pv(pi_, pair, e_):
                        for h_, kt in enumerate(pair):
                            ki = pi_ * 2 + h_
                            for qs in range(nq):
                                mm(psb[4 + qs][:, 0:129], ET[e_][:, h_ * 512 + qs * 128:h_ * 512 + (qs + 1) * 128], Vaug[:, kt, hd, :],
                                   ki == 0, ki == len(ktiles) - 1, ["ET%d" % e_], ["ps%d" % (4 + qs)])
                    prev = None
                    for pi_, pair in enumerate(pairs):
                        sb = si % 2; si += 1
                        for h_, kt in enumerate(pair):
                            mm(pp[sb][:, h_ * 512:h_ * 512 + qw], KT[:, hd, kt * 128:(kt + 1) * 128], Qm[qsl][comp][:, q0:q0 + qw],
                               True, True, [("Qm", qsl, comp)], ["pS%d" % sb])
                        e_ = ei % 3; ei += 1
                        in_ap = pp[sb][:, :].rearrange("p (a n) -> p a n", a=2)[:, 0:len(pair), 0:qw]
                        out_ap = ET[e_][:, :].rearrange("p (a n) -> p a n", a=2)[:, 0:len(pair), 0:qw]
                        act(out_ap, in_ap, AF.Exp, ["pS%d" % sb], ["ET%d" % e_], scale=0.125)
                        if prev is not None:
                            pv(*prev)
                        prev = (pi_, pair, e_)
                    pv(*prev)
                    a = ai % 2; ai += 1
                    for qs in range(nq):
                        cp("vector", accs[a][:, qs, :], psb[4 + qs][:, 0:129], ["ps%d" % (4 + qs)], [("accs", a, qs)])
                    ak = [("accs", a, qs) for qs in range(nq)]
                    P.op("vector", lambda e, a=a, nq=nq: e.reciprocal(out=rs[a][:, 0:nq], in_=accs[a][:, 0:nq, 128]), ak, ["rs%d" % a])
                    if comp == 0:
                        tt("vector", O1[:, 0:nq, :], accs[a][:, 0:nq, 0:128], rs[a][:, 0:nq].unsqueeze(2).to_broadcast([128, nq, 128]), ALU.mult,
                           ak + ["rs%d" % a], ["O1"])
                    else:
                        ts("vector", rs2[a][:, 0:nq], rs[a][:, 0:nq], neglam[:, 0:1], None, ALU.mult, None, ["rs%d" % a, "neglam"], ["rs2%d" % a])
                        tt("vector", od[a][:, 0:nq, :], accs[a][:, 0:nq, 0:128], rs2[a][:, 0:nq].unsqueeze(2).to_broadcast([128, nq, 128]), ALU.mult,
                           ak + ["rs2%d" % a], ["od%d" % a])
                        tt("vector", od[a][:, 0:nq, :], od[a][:, 0:nq, :], O1[:, 0:nq, :], ALU.add, ["od%d" % a, "O1"], ["od%d" % a])
                        act(junk[a][:, 0:nq, :], od[a][:, 0:nq, :], AF.Square, ["od%d" % a], ["junk%d" % a])
                        P.op("vector", lambda e, a=a, nq=nq: e.reduce_sum(out=ss2[a][:, 0:nq], in_=junk[a][:, 0:nq, :], axis=AX.X),
                             ["junk%d" % a], ["ss2%d" % a])
                        ts("vector", r2[a][:, 0:nq], ss2[a][:, 0:nq], 1.0 / 128, EPS, ALU.mult, ALU.add, ["ss2%d" % a], ["r2%d" % a])
                        act(r2[a][:, 0:nq], r2[a][:, 0:nq], AF.Sqrt, ["r2%d" % a], ["r2%d" % a])
                        P.op("vector", lambda e, a=a, nq=nq: e.reciprocal(out=r2[a][:, 0:nq], in_=r2[a][:, 0:nq]), ["r2%d" % a], ["r2%d" % a])
                        tt("vector", od[a][:, 0:nq, :], od[a][:, 0:nq, :], r2[a][:, 0:nq].unsqueeze(2).to_broadcast([128, nq, 128]), ALU.mult,
                           ["od%d" % a, "r2%d" % a], ["od%d" % a])
                        tt("vector", ON[:, qt0:qt0 + nq, hd * 128:(hd + 1) * 128], od[a][:, 0:nq, :],
                           sg[:, :].unsqueeze(1).to_broadcast([128, nq, 128]), ALU.mult, ["od%d" % a, "sg"], [("ON", qt0, hd)])
        P.barrier()
        if attn_stop == "core":
            return
        A.reset(m2)
        wo = A.tile([128, 8, D], BF16, "wo")
        cast_rows(wo, w_o, 0, D, "wo")
        out_proj(lambda t, slot: ON[:, t, :], None, wo, l, list(range(NT)), transpose=True)

    def out_proj(src_fn, zT, wo, l, tiles, transpose):
        g1 = {}
        for s in ((0, 1) if len(tiles) > NTL else (0,)):
            g1[s] = A.tile([128, D], F32, "g1bc")
            bcload(g1[s], modrow[l, s:s + 1, 2 * D:3 * D], D, "g1bc%d" % s)
        ONT = [A.tile([128, 8, 128], BF16, "ONT") for _ in range(2)]
        ht = [A.tile([128, D], F32, "ht") for _ in range(2)]
        tmp = [A.tile([128, D], F32, "tmp") for _ in range(2)]
        it = 0
        for i, t in enumerate(tiles):
            s = i % 2
            st = 0 if t < NTL else 1
            dma("sync" if s == 0 else "scalar", ht[s][:], h[t * 128:(t + 1) * 128, :], "ht%d" % s, [], ["ht%d" % s])
            if transpose:
                transpose_to(src_fn(t, s), 128, ONT[s][:], 2 + s, [], ["ONT%d" % s])
            for n in range(2):
                b = it % 2; it += 1
                for j in range(8):
                    lhs = ONT[s][:, j, :] if transpose else zT[:, j, t * 128:(t + 1) * 128]
                    mm(psb[b][:, :], lhs, wo[:, j, n * 512:(n + 1) * 512], j == 0, j == 7,
                       [("wo", j, 0)] + (["ONT%d" % s] if transpose else []), ["ps%d" % b])
                tt("vector", tmp[s][:, n * 512:(n + 1) * 512], psb[b][:, :], g1[st][:, n * 512:(n + 1) * 512], ALU.mult,
                   ["ps%d" % b, "g1bc%d" % st], ["tmp%d%d" % (s, n)])
                tt("vector", ht[s][:, n * 512:(n + 1) * 512], ht[s][:, n * 512:(n + 1) * 512], tmp[s][:, n * 512:(n + 1) * 512], ALU.add,
                   ["tmp%d%d" % (s, n), "ht%d" % s], ["ht%d" % s])
            dma("sync" if s == 0 else "scalar", h[t * 128:(t + 1) * 128, :], ht[s][:], "ht%d" % s, ["ht%d" % s], [])

    def phase_moe(l, with_ctx):
        A.reset(base_mark)
        sets = (0, 1) if with_ctx else (0,)
        tiles = list(range(NT if with_ctx else NTL))
        ncap = [256, 32]
        probsT = A.tile([16, 2304], F32, "probsT")
        wr = A.tile([128, 8, NE], BF16, "wr")
        cast_rows(wr, w_router[l], 0, NE, "wr")
        if l == 0:
            precast(1, [5])
        idxT = A.tile([128, 3, NE], I32, "idxT"); gateT = A.tile([128, 3, NE], F32, "gateT")
        g2 = {}
        for s in sets:
            g2[s] = A.tile([128, D], F32, "g2bc")
            bcload(g2[s], modrow[l, s:s + 1, 5 * D:6 * D], D, "g2bc%d" % s)
        m0 = A.mark()
        gsh = norm_setup(l, 2, sets)
        nbuf = norm_alloc()
        nTt = [A.tile([128, 8, 128], BF16, "nTt") for _ in range(2)]
        lg = [A.tile([128, NE], F32, "lg") for _ in range(2)]
        mxl = [A.tile([128, 1], F32, "mxl") for _ in range(2)]
        sme = [A.tile([128, 1], F32, "sme") for _ in range(2)]
        for i, t in enumerate(tiles):
            s = norm_tile(nbuf, i, t, gsh[0 if t < NTL else 1])
            k = "r%d" % s
            dma("sync", mbf[t * 128:(t + 1) * 128, :], nbuf["nb"][s][:], "n%dnb" % s, ["n%dnb" % s], [("mbf", t)])
            transpose_to(nbuf["nb"][s][:], 128, nTt[s][:], 4 + s, ["n%dnb" % s], [k + "nTt"])
            for j in range(8):
                mm(psb[6 + s][:, 0:NE], nTt[s][:, j, :], wr[:, j, :], j == 0, j == 7, [k + "nTt", ("wr", j, 0)], ["ps%d" % (6 + s)])
            P.op("vector", lambda e, s=s: e.reduce_max(out=mxl[s][:], in_=psb[6 + s][:, 0:NE], axis=AX.X), ["ps%d" % (6 + s)], [k + "mx"])
            ts("vector", mxl[s][:], mxl[s][:], -1.0, None, ALU.mult, None, [k + "mx"], [k + "mx"])
            act(lg[s][:], psb[6 + s][:, 0:NE], AF.Exp, ["ps%d" % (6 + s), k + "mx"], [k + "lg", k + "sm"], bias=mxl[s][:, 0:1], accum_out=sme[s][:])
            P.op("vector", lambda e, s=s: e.reciprocal(out=sme[s][:], in_=sme[s][:]), [k + "sm"], [k + "sm"])
            ts("vector", lg[s][:], lg[s][:], sme[s][:, 0:1], None, ALU.mult, None, [k + "lg", k + "sm"], [k + "lg"])
            tr(psb[s][0:NE, 0:128], lg[s][:], ident_f[:], [k + "lg", "identf"], ["ps%d" % s])
            cp("vector", probsT[:, t * 128:(t + 1) * 128], psb[s][0:NE, 0:128], ["ps%d" % s], [("pT", t)])
        P.barrier()
        if l == 0:
            precast(1, [6])
        A.reset(m0)
        work = A.tile([16, 2048], F32, "work")
        gate = A.tile([16, 288], F32, "gate"); idxu = A.tile([16, 288], U32, "idxu"); idxf = A.tile([16, 288], F32, "idxf")
        for s in sets:
            n0, ntok, cap, c0 = (0, 2048, 256, 0) if s == 0 else (2048, 256, 32, 256)
            cp("vector", work[:, 0:ntok], probsT[:, n0:n0 + ntok], [], ["work"])
            for r in range(cap // 8):
                g_ = gate[:, c0 + r * 8:c0 + (r + 1) * 8]
                P.op("vector", lambda e, g_=g_, ntok=ntok: e.max(out=g_, in_=work[:, 0:ntok]), ["work"], ["gate"])
                P.op("vector", lambda e, g_=g_, ntok=ntok, r=r, c0=c0: e.max_index(out=idxu[:, c0 + r * 8:c0 + (r + 1) * 8], in_max=g_, in_values=work[:, 0:ntok]),
                     ["work", "gate"], ["idxu"])
                P.op("vector", lambda e, g_=g_, ntok=ntok: e.match_replace(out=work[:, 0:ntok], in_to_replace=g_, in_values=work[:, 0:ntok], imm_value=-1.0),
                     ["work", "gate"], ["work"])
        ncol = 288 if with_ctx else 256
        cp("vector", idxf[:, 0:ncol], idxu[:, 0:ncol], ["idxu"], ["idxf"])
        if with_ctx:
            ts("vector", idxf[:, 256:288], idxf[:, 256:288], 2048.0, None, ALU.add, None, ["idxf"], ["idxf"])
        parts = [(0, 0, 128), (1, 128, 128)] + ([(2, 256, 32)] if with_ctx else [])
        for (pi, c0, rows) in parts:
            tr(psb[0][0:rows, 0:NE], idxf[:, c0:c0 + rows], ident_f[0:NE, 0:NE], ["idxf", "identf"], ["ps0"])
            cp("vector", idxT[0:rows, pi, :], psb[0][0:rows, 0:NE], ["ps0"], ["idxT"])
            tr(psb[1][0:rows, 0:NE], gate[:, c0:c0 + rows], ident_f[0:NE, 0:NE], ["gate", "identf"], ["ps1"])
            cp("vector", gateT[0:rows, pi, :], psb[1][0:rows, 0:NE], ["ps1"], ["gateT"])
        P.barrier()
        A.reset(m0)
        ntk = 288 if with_ctx else 256
        Xg = [[A.tile([128, D], BF16, "Xg") for _ in parts] for _ in range(2)]
        XgT = [A.tile([128, 8, ntk], BF16, "XgT") for _ in range(2)]
        NWG = 4
        wg = [A.tile([128, 8, 1024], BF16, "wg") for _ in range(NWG)]
        wd = [A.tile([128, 2, D], BF16, "wd") for _ in range(NWG)]
        hT = A.tile([128, 22, ntk], BF16, "hT")
        sgt = [A.tile([128, ntk], F32, "sgt") for _ in range(2)]
        ysb = [A.tile([128, D], F32, "ysb") for _ in range(len(parts) * 2)]

        def gather(e):
            sl = e % 2
            for (pi, c0, rows) in parts:
                P.dma("gpsimd", lambda en, pi=pi, rows=rows, sl=sl, e=e: en.indirect_dma_start(
                    out=Xg[sl][pi][0:rows, :], out_offset=None, in_=mbf[:, :],
                    in_offset=bass.IndirectOffsetOnAxis(ap=idxT[0:rows, pi, e:e + 1], axis=0)),
                    "Xg%d%d" % (sl, pi), [], ["Xg%d%d" % (sl, pi)])

        def xpose(e):
            sl = e % 2
            for (pi, c0, rows) in parts:
                transpose_to(Xg[sl][pi][0:rows, :], rows, XgT[sl][:, :, c0:c0 + rows], 2 + pi % 2,
                             ["Xg%d%d" % (sl, pi)], [("XgT", sl, pi)], eng="vector")

        wgi = [0]; wdi = [0]; gi = [0]; yi = [0]

        def load_wg(e, b):
            s = wgi[0] % NWG; wgi[0] += 1
            nf = 512 if b < 5 else 256
            keys = []
            if e < NPRE[l]:
                vg = wgu_bf[l, e].rearrange("(j p) f -> p j f", p=128)
                kg = [("wg%d" % s, j, 0) for j in range(8)]; ku = [("wg%d" % s, j, 512) for j in range(8)]
                dma("sync", wg[s][:, :, 0:nf], vg[:, :, b * 512:b * 512 + nf], "wgh%d" % s, [], kg)
                dma("sync", wg[s][:, :, 512:512 + nf], vg[:, :, FE + b * 512:FE + b * 512 + nf], "wgh%d" % s, [], ku)
                cast_finalize("wgh%d" % s, kg + ku)
            else:
                cast_rows(wg[s], w_gu[l, e], b * 512, nf, "wg%d" % s, 0, defer=keys)
                cast_rows(wg[s], w_gu[l, e], FE + b * 512, nf, "wg%d" % s, 512, defer=keys)
                cast_finalize("wg%d" % s, keys)
            return s

        def load_wd(e, b):
            s = wdi[0] % NWG; wdi[0] += 1
            if e < NPRE[l]:
                vd = wdn_bf[l, e].rearrange("(c p) d -> p c d", p=128)
                kd = [("wd%d" % s, 0, 0), ("wd%d" % s, 1, 0)]
                dma("sync", wd[s][:], vd[:, 2 * b:2 * b + 2, :], "wdh%d" % s, [], kd)
                cast_finalize("wdh%d" % s, kd)
            else:
                cast_rows(wd[s], w_dn[l, e][2 * b * 128:(2 * b + 2) * 128, :], 0, D, "wd%d" % s)
            return s

        blocks = []
        for e in range(NE):
            blocks += [("g", e, b) for b in range(6)] + [("d", e, b) for b in range(11)]
        PF = 3
        slots = {}
        gather(0)
        xpose(0)
        for bi in range(min(PF, len(blocks))):
            kind, e, b = blocks[bi]
            slots[bi] = load_wg(e, b) if kind == "g" else load_wd(e, b)
        for bi, (kind, e, b) in enumerate(blocks):
            nb_ = bi + PF
            if nb_ < len(blocks):
                k2, e2, b2 = blocks[nb_]
                slots[nb_] = load_wg(e2, b2) if k2 == "g" else load_wd(e2, b2)
            sl = e % 2
            xk = [("XgT", sl, pi) for (pi, _, _) in parts]
            if kind == "g":
                if b == 0 and e + 1 < NE:
                    gather(e + 1)
                s = slots[bi]
                for sub in range(4 if b < 5 else 2):
                    c = b * 4 + sub
                    gb = gi[0] % 2; gi[0] += 1
                    for gu, colb in ((0, sub * 128), (1, 512 + sub * 128)):
                        bank = gb * 2 + gu
                        for j in range(8):
                            mm(psb[bank][:, 0:ntk], wg[s][:, j, colb:colb + 128], XgT[sl][:, j, :], j == 0, j == 7,
                               [("wg%d" % s, j, 0 if gu == 0 else 512)] + xk, ["ps%d" % bank])
                    act(sgt[gb][:], psb[gb * 2][:, 0:ntk], AF.Silu, ["ps%d" % (gb * 2)], ["sgt%d" % gb])
                    tt("vector", hT[:, c, :], sgt[gb][:], psb[gb * 2 + 1][:, 0:ntk], ALU.mult,
                       ["sgt%d" % gb, "ps%d" % (gb * 2 + 1)], [("hT", c)])
            else:
                s = slots[bi]
                if b == 0 and e + 1 < NE:
                    xpose(e + 1)
                for cc in range(2):
                    c = 2 * b + cc
                    for (pi, c0, rows) in parts:
                        for dh in range(2):
                            bank = (4 + pi * 2 + dh) if pi < 2 else dh
                            mm(psb[bank][0:rows, :], hT[:, c, c0:c0 + rows], wd[s][:, cc, dh * 512:(dh + 1) * 512],
                               c == 0, c == 21, [("wd%d" % s, cc, 0), ("hT", c)], ["ps%d" % bank])
                if b == 10:
                    for (pi, c0, rows) in parts:
                        yb = yi[0] % len(ysb); yi[0] += 1
                        st = 0 if pi < 2 else 1
                        for dh in range(2):
                            bank = (4 + pi * 2 + dh) if pi < 2 else dh
                            stt("vector", ysb[yb][0:rows, dh * 512:(dh + 1) * 512], psb[bank][0:rows, :], gateT[0:rows, pi, e:e + 1],
                                g2[st][0:rows, dh * 512:(dh + 1) * 512], ALU.mult, ALU.mult,
                                ["ps%d" % bank, "gateT", "g2bc%d" % st], ["ysb%d" % yb])
                        P.dma("gpsimd", lambda en, pi=pi, rows=rows, yb=yb, e=e: en.indirect_dma_start(
                            out=h[:, :], out_offset=bass.IndirectOffsetOnAxis(ap=idxT[0:rows, pi, e:e + 1], axis=0),
                            in_=ysb[yb][0:rows, :], in_offset=None, compute_op=ALU.add),
                            "ysb%d" % yb, ["ysb%d" % yb] + [("hsc", e - 1, p2) for (p2, _, _) in parts], [("hsc", e, pi)])

    def phase_lru(l=1):
        A.reset(base_mark)
        nT = A.tile([128, 8, 2304], BF16, "nT")
        zT = A.tile([128, 8, 2048], BF16, "zT")
        m1 = A.mark()
        gsh = norm_setup(l, 1, (0, 1))
        nbuf = norm_alloc()
        for i, t in enumerate(range(NT)):
            s = norm_tile(nbuf, i, t, gsh[0 if t < NTL else 1])
            transpose_to(nbuf["nb"][s][:], 128, nT[:, :, t * 128:(t + 1) * 128], 4 + i % 2, ["n%dnb" % s], [("nT", t)],
                         eng="scalar" if i % 2 else "vector")
        P.barrier()
        A.reset(m1)
        nT_all = []
        win = A.tile([128, 8, 2 * D], BF16, "win")
        cast_rows(win, lru_w_in, 0, 2 * D, "win")
        wgt = A.tile([128, 2, 8, 256], BF16, "wgt")
        for d_ in range(2):
            for k_ in range(8):
                dma("gpsimd", wgt[:, d_, k_, :], w_gates[d_, k_], "wgt", [], [("wgt", d_, k_)])
        cast_finalize("wgt", [("wgt", d_, k_) for d_ in range(2) for k_ in range(8)])
        bin_ = A.tile([128, 16], F32, "bin")
        dma("sync", bin_[:], lru_b_in.rearrange("(j p) -> p j", p=128), "bin", [], ["bin"], allow_slow_non_contiguous=True)
        cw = A.tile([128, 4, 8], F32, "cw")
        dma("sync", cw[:], conv_w.rearrange("j (k p) -> p j k", p=128), "cw", [], ["cw"], allow_slow_non_contiguous=True)
        cb = A.tile([128, 8], F32, "cb")
        dma("sync", cb[:], conv_b.rearrange("(k p) -> p k", p=128), "cb", [], ["cb"], allow_slow_non_contiguous=True)
        bg = A.tile([128, 2, 8, 2], F32, "bg")
        dma("sync", bg[:], b_gates.rearrange("d k (hf p) -> p d k hf", p=128), "bg", [], ["bg"], allow_slow_non_contiguous=True)
        lamt = A.tile([128, 2, 8], F32, "lamt")
        dma("sync", lamt[:], lru_lam.rearrange("d (k p) -> p d k", p=128), "lamt", [], ["lamt"], allow_slow_non_contiguous=True)
        sdec = A.tile([128, 2, 8], F32, "sdec"); sdec2 = A.tile([128, 2, 8], F32, "sdec2")
        act(sdec[:], lamt[:], AF.Exp, ["lamt"], ["sdec"], scale=-1.0)
        act(sdec[:], sdec[:], AF.Ln, ["sdec"], ["sdec"], bias=1.0)
        ts("vector", sdec2[:], sdec[:], -16.0, None, ALU.mult, None, ["sdec"], ["sdec2"])
        ts("vector", sdec[:], sdec[:], -8.0, None, ALU.mult, None, ["sdec", "sdec2"], ["sdec"])
        LC, LL = 256, 2048
        xpc = A.tile([128, LC + 3], F32, "xpc"); xpl = A.tile([128, LL + 3], F32, "xpl")
        xc = A.tile([128, 2304], F32, "xc")
        xb = A.tile([128, 2304], BF16, "xb")
        yg = A.tile([128, 2048], F32, "yg")
        rg = A.tile([128, 2304], F32, "rg"); ig = A.tile([128, 2304], F32, "ig")
        at = A.tile([128, 2304], F32, "at"); qt_ = A.tile([128, 2304], F32, "qt")
        hf = A.tile([128, 2304], F32, "hf"); hb = A.tile([128, 2304], F32, "hb")
        for tl in (xpc, xpl):
            P.op("gpsimd", lambda e, tl=tl: e.memset(tl[:], 0.0), [], ["xp"])
        precast(1, range(5))
        it = 0
        chunks = [(n * 512, 512) for n in range(4)] + [(2048, 256)]
        for k in range(8):
            for (n0, nw) in chunks:
                b = it % 2; it += 1
                for j in range(8):
                    mm(psb[b][:, 0:nw], win[:, j, D + k * 128:D + (k + 1) * 128], nT[:, j, n0:n0 + nw], j == 0, j == 7, [("win", j, 1024)], ["ps%d" % b])
                dst = xpl[:, 2 + n0:2 + n0 + nw] if n0 < 2048 else xpc[:, 2:2 + LC]
                act(dst, psb[b][:, 0:nw], AF.Identity, ["ps%d" % b, "bin"], ["xp"], bias=bin_[:, 8 + k:9 + k])
                if n0 < 2048:
                    b = it % 2; it += 1
                    for j in range(8):
                        mm(psb[b][:, 0:nw], win[:, j, k * 128:(k + 1) * 128], nT[:, j, n0:n0 + nw], j == 0, j == 7, [("win", j, 0)], ["ps%d" % b])
                    act(yg[:, n0:n0 + nw], psb[b][:, 0:nw], AF.Gelu_apprx_tanh, ["ps%d" % b, "bin"], ["yg"], bias=bin_[:, k:k + 1])
            for (xp, o0, L) in ((xpc, 0, LC), (xpl, LC, LL)):
                ts("vector", xc[:, o0:o0 + L], xp[:, 0:L], cw[:, 0, k:k + 1], cb[:, k:k + 1], ALU.mult, ALU.add, ["xp", "cw", "cb"], ["xc"])
                for j in range(1, 4):
                    stt("vector", xc[:, o0:o0 + L], xp[:, j:j + L], cw[:, j, k:k + 1], xc[:, o0:o0 + L], ALU.mult, ALU.add, ["xp", "cw", "xc"], ["xc"])
            cp("vector", xb[:], xc[:], ["xc"], ["xb"])
            for d in range(2):
                for half, dstt, dk in ((0, rg, "rg"), (1, ig, "ig")):
                    for (n0, nw) in [(0, 256)] + [(256 + n * 512, 512) for n in range(4)]:
                        b = it % 2; it += 1
                        mm(psb[b][:, 0:nw], wgt[:, d, k, half * 128:(half + 1) * 128], xb[:, n0:n0 + nw], True, True, [("wgt", d, k), "xb"], ["ps%d" % b])
                        act(dstt[:, n0:n0 + nw], psb[b][:, 0:nw], AF.Sigmoid, ["ps%d" % b, "bg"], [dk], bias=bg[:, d, k, half:half + 1])
                act(at[:], rg[:], AF.Exp, ["rg", "sdec"], ["at"], scale=sdec[:, d, k:k + 1])
                act(qt_[:], rg[:], AF.Exp, ["rg", "sdec2"], ["qt"], scale=sdec2[:, d, k:k + 1])
                act(qt_[:], qt_[:], AF.Sqrt, ["qt"], ["qt"], scale=-1.0, bias=1.0)
                tt("vector", ig[:], ig[:], xc[:], ALU.mult, ["ig", "xc"], ["ig"])
                tt("vector", ig[:], ig[:], qt_[:], ALU.mult, ["ig", "qt"], ["ig"])
                hh, hk = (hf, "hf") if d == 0 else (hb, "hb")
                if d == 0:
                    P.op("vector", lambda e, hh=hh: e.tensor_tensor_scan(out=hh[:, 0:LC], data0=at[:, 0:LC], data1=ig[:, 0:LC], initial=0.0,
                                                                     op0=ALU.mult, op1=ALU.add), ["at", "ig"], [hk])
                    P.op("vector", lambda e, hh=hh: e.tensor_tensor_scan(out=hh[:, LC:], data0=at[:, LC:], data1=ig[:, LC:], initial=hh[:, LC - 1:LC],
                                                                     op0=ALU.mult, op1=ALU.add), ["at", "ig", hk], [hk])
                else:
                    P.op("vector", lambda e, hh=hh: e.tensor_tensor_scan(out=hh[:, 0:LC][:, ::-1], data0=at[:, 0:LC][:, ::-1], data1=ig[:, 0:LC][:, ::-1],
                                                                     initial=0.0, op0=ALU.mult, op1=ALU.add), ["at", "ig"], [hk])
                    P.op("vector", lambda e, hh=hh: e.tensor_tensor_scan(out=hh[:, LC:][:, ::-1], data0=at[:, LC:][:, ::-1], data1=ig[:, LC:][:, ::-1],
                                                                     initial=hh[:, 0:1], op0=ALU.mult, op1=ALU.add), ["at", "ig", hk], [hk])
            tt("vector", hf[:, LC:], hf[:, LC:], hb[:, LC:], ALU.add, ["hf", "hb"], ["hf"])
            tt("vector", zT[:, k, :], hf[:, LC:], yg[:], ALU.mult, ["hf", "yg"], [("zT", k)])
        P.barrier()
        A.reset(m1)
        wo = A.tile([128, 8, D], BF16, "wo")
        cast_rows(wo, lru_w_out, 0, D, "wo")
        out_proj(None, zT, wo, l, list(range(NTL)), transpose=False)

    def phase_final():
        A.reset(base_mark)
        fg = A.tile([128, D], F32, "fg")
        bcload(fg, final_g, D, "fg")
        xt = [A.tile([128, D], F32, "fxt") for _ in range(2)]
        yo = [A.tile([128, D], F32, "fyo") for _ in range(2)]
        jk = [A.tile([128, D], BF16, "fjk") for _ in range(2)]
        ss = [A.tile([128, 1], F32, "fss") for _ in range(2)]
        rstd = [A.tile([128, 1], F32, "frs") for _ in range(2)]
        for t in range(NTL):
            s = t % 2; k = "f%d" % s
            dma("sync" if s == 0 else "scalar", xt[s][:], h[t * 128:(t + 1) * 128, :], k + "xt", [], [k + "xt"])
            act(jk[s][:], xt[s][:], AF.Square, [k + "xt"], [k + "jk", k + "ss"], accum_out=ss[s][:])
            rstd_from_ss(ss[s][:], rstd[s][:], D, k)
            stt("vector", yo[s][:], xt[s][:], rstd[s][:, 0:1], fg[:], ALU.mult, ALU.mult, [k + "xt", k + "rstd", "fg"], [k + "yo"])
            dma("sync" if s == 0 else "scalar", out[t * 128:(t + 1) * 128, :], yo[s][:], k + "yo", [k + "yo"], [])

    dma("sync", h[0:2048, :], x_in, "hinit0", [], [])
    dma("scalar", h[2048:2304, :], ctx_in, "hinit1", [], [])
    stages = ["mod", "attn", "moe0", "lru", "moe1", "final"]
    nst = len(stages) if stop_after is None else stages.index(stop_after) + 1
    for st in stages[:nst]:
        if st == "mod":
            phase_mod()
        elif st == "attn":
            phase_attn(0)
        elif st == "moe0":
            phase_moe(0, True)
        elif st == "lru":
            phase_lru(1)
        elif st == "moe1":
            phase_moe(1, False)
        elif st == "final":
            phase_final()
        P.barrier()
    P.wait_all("sync")
    P.emit()
    return nc


_CONST = {}


def make_in_maps(inputs):
    if "C" not in _CONST:
        C, S, perm = rope_tables()
        _CONST.update(C=C, S=S, perm=perm, ident=np.eye(128, dtype=np.float32))
    g = lambda k: np.ascontiguousarray(inputs[k])
    shared = {
        "c_ctx": g("c_ctx"), "ada_w": g("ada_w"), "ada_b": g("ada_b"), "norm1_g": g("norm1_g"), "norm2_g": g("norm2_g"),
        "final_g": g("final_g").reshape(1, D), "attn_w_qkv": g("attn_w_qkv")[0],
        "attn_lq1": g("attn_lq1"), "attn_lk1": g("attn_lk1"), "attn_lq2": g("attn_lq2"), "attn_lk2": g("attn_lk2"),
        "attn_subln_g": g("attn_subln_g"), "attn_w_o": g("attn_w_o")[0],
        "lru_w_in": g("lru_w_in")[0], "lru_b_in": g("lru_b_in")[0], "lru_conv_w": g("lru_conv_w")[0], "lru_conv_b": g("lru_conv_b")[0],
        "lru_w_gates": g("lru_w_gates")[0], "lru_b_gates": g("lru_b_gates")[0], "lru_lambda": g("lru_lambda")[0],
        "lru_w_out": g("lru_w_out")[0], "moe_w_router": g("moe_w_router"), "moe_w_gate_up": g("moe_w_gate_up"),
        "moe_w_down": g("moe_w_down"),
        "rope_c": _CONST["C"], "rope_s": _CONST["S"], "perm": _CONST["perm"], "ident": _CONST["ident"],
    }
    maps = []
    for b in range(8):
        m = dict(shared)
        m["x"] = g("x")[b]; m["ctx"] = g("ctx")[b]; m["c"] = g("c")[b]
        maps.append(m)
    return maps


def kernel(**inputs):
    nc = build_program()
    in_maps = make_in_maps(inputs)
    res = run_bass_kernel_spmd(nc, in_maps, core_ids=list(range(8)))
    return np.stack([np.asarray(r["out"]) for r in res.results], axis=0).astype(np.float32)
```
